# Optimizing a Trainium2 kernel written in Bass

```python
import jax, jax.numpy as jnp
from jax import lax
import numpy as np

D_MODEL = 1024
BATCH = 16
SEQ = 2048
DEPTH = 2

GRID_W = 64
CTX_LEN = 256
N_MIXERS = 2
POOL_WINDOWS = (2, 4, 8, 16)
N_POOL_GROUPS = 4
POOL_GC = D_MODEL // N_POOL_GROUPS
D_RNN = (4 * D_MODEL // 3) // 128 * 128
LRU_BLOCK = 128
LRU_HEADS = D_RNN // LRU_BLOCK
CONV_W = 4
LRU_C = 8.0
D_FF = (8 * D_MODEL // 3 + 255) // 256 * 256
N_EXPERTS = 8
TOP_K = 2
D_EXPERT = 7 * D_MODEL // 2
MOE_BLOCK = 256
EPS = 1e-6

kernel_name = "hybrid_pool_rglru_moe_dit_trunk"


def rmsnorm(x, g):
    xf = x.astype(jnp.float32)
    y = xf * lax.rsqrt(jnp.mean(xf * xf, axis=-1, keepdims=True) + EPS)
    return (y * g.astype(jnp.float32)).astype(x.dtype)


def modulate(h, shift, scale):
    return h * (1 + scale) + shift


def centred_window_mean(u, win, axis):
    n = u.shape[axis]
    cs = jnp.cumsum(u.astype(jnp.float32), axis=axis)
    pad = [(0, 0)] * u.ndim
    pad[axis] = (1, 0)
    cs = jnp.pad(cs, pad)
    t = np.arange(n)
    lo = np.clip(t - win // 2, 0, n)
    hi = np.clip(t - win // 2 + win, 0, n)
    shape = [1] * u.ndim
    shape[axis] = n
    count = jnp.asarray((hi - lo).astype(np.float32)).reshape(shape)
    s = jnp.take(cs, jnp.asarray(hi), axis=axis) - jnp.take(cs, jnp.asarray(lo), axis=axis)
    return (s / count).astype(u.dtype)


def pool_mixer(h, axis, w, scale):
    groups = jnp.split(h, N_POOL_GROUPS, axis=-1)
    pooled = jnp.stack([centred_window_mean(g, win, axis) - g
                        for g, win in zip(groups, POOL_WINDOWS)], axis=-2)
    y = jnp.einsum('...gc,gcd->...gd', pooled, w)
    return y.reshape(h.shape) * scale


def centred_depthwise_conv(u, w, b):
    L = u.shape[1]
    left = CONV_W // 2
    right = CONV_W - 1 - left
    up = jnp.pad(u, ((0, 0), (left, right), (0, 0)))
    return sum(up[:, k:k + L] * w[k] for k in range(CONV_W)) + b


def lru_inputs(h, w_in, conv_w, conv_b):
    gate, u = jnp.split(h @ w_in, 2, axis=-1)
    return gate, centred_depthwise_conv(u, conv_w, conv_b)


def _scan_combine(left, right):
    a_l, b_l = left
    a_r, b_r = right
    return a_l * a_r, a_r * b_l + b_r


def rglru_scan(u, w_r, b_r, w_i, b_i, lam, h0, reverse):
    bsz, L, _ = u.shape
    ub = u.reshape(bsz, L, LRU_HEADS, LRU_BLOCK)
    r = jax.nn.sigmoid(jnp.einsum('blhc,hcd->blhd', ub, w_r).reshape(bsz, L, D_RNN) + b_r)
    i = jax.nn.sigmoid(jnp.einsum('blhc,hcd->blhd', ub, w_i).reshape(bsz, L, D_RNN) + b_i)
    log_a = -LRU_C * r.astype(jnp.float32) * jax.nn.softplus(-lam.astype(jnp.float32))
    a = jnp.exp(log_a)
    b = jnp.sqrt(-jnp.expm1(2.0 * log_a)) * (i * u).astype(jnp.float32)
    if reverse:
        a, b = jnp.flip(a, 1), jnp.flip(b, 1)
    a_cum, h = lax.associative_scan(_scan_combine, (a, b), axis=1)
    h = h + a_cum * h0[:, None, :]
    if reverse:
        h = jnp.flip(h, 1)
    return h


def rglru_mixer(hx, hz, w_in, conv_w, conv_b, w_r, b_r, w_i, b_i, lam, w_out, need_ctx_out):
    gx, ux = lru_inputs(hx, w_in, conv_w, conv_b)
    gz, uz = lru_inputs(hz, w_in, conv_w, conv_b)
    h0 = jnp.zeros((uz.shape[0], D_RNN), jnp.float32)
    sum_x = 0.0
    sum_z = 0.0
    for d, reverse in enumerate((False, True)):
        hz_d = rglru_scan(uz, w_r[d], b_r[d], w_i[d], b_i[d], lam[d], h0, reverse)
        state = hz_d[:, 0] if reverse else hz_d[:, -1]
        hx_d = rglru_scan(ux, w_r[d], b_r[d], w_i[d], b_i[d], lam[d], state, reverse)
        sum_x = sum_x + hx_d
        sum_z = sum_z + hz_d
    yx = (sum_x.astype(hx.dtype) * jax.nn.gelu(gx)) @ w_out
    yz = (sum_z.astype(hz.dtype) * jax.nn.gelu(gz)) @ w_out if need_ctx_out else None
    return yx, yz


def swiglu(h, w_gu, w_down):
    g, u = jnp.split(h @ w_gu, 2, axis=-1)
    return (jax.nn.silu(g) * u) @ w_down


def moe_swiglu(h, w_router, w_gu, w_down):
    shp = h.shape
    t = h.reshape(-1, shp[-1])
    n = t.shape[0]
    logits = t.astype(jnp.float32) @ w_router.astype(jnp.float32)
    probs = jax.nn.softmax(logits, axis=-1)
    top_p, top_e = lax.top_k(probs, TOP_K)
    top_w = (top_p / jnp.sum(top_p, axis=-1, keepdims=True)).astype(h.dtype)
    flat_e = top_e.reshape(-1)
    flat_w = top_w.reshape(-1)
    flat_tok = jnp.repeat(jnp.arange(n, dtype=jnp.int32), TOP_K)
    order = jnp.argsort(flat_e)
    se = flat_e[order]
    counts = jnp.bincount(flat_e, length=N_EXPERTS)
    padded = (counts + MOE_BLOCK - 1) // MOE_BLOCK * MOE_BLOCK
    starts = jnp.cumsum(counts) - counts
    pends = jnp.cumsum(padded)
    pstarts = pends - padded
    rank = jnp.arange(n * TOP_K, dtype=jnp.int32) - starts[se]
    dest = pstarts[se] + rank
    n_slots = (-(-(n * TOP_K) // MOE_BLOCK) + N_EXPERTS) * MOE_BLOCK
    n_blocks = n_slots // MOE_BLOCK
    slot_tok = jnp.full((n_slots,), n, jnp.int32).at[dest].set(flat_tok[order])
    slot_w = jnp.zeros((n_slots,), h.dtype).at[dest].set(flat_w[order])
    block_e = jnp.minimum(jnp.searchsorted(pends, jnp.arange(n_blocks, dtype=jnp.int32) * MOE_BLOCK,
                                           side='right'), N_EXPERTS - 1)
    t_pad = jnp.concatenate([t, jnp.zeros((1, t.shape[1]), t.dtype)], axis=0)

    def expert_block(args):
        tok, e = args
        return swiglu(t_pad[tok], w_gu[e], w_down[e])

    yb = lax.map(expert_block, (slot_tok.reshape(n_blocks, MOE_BLOCK), block_e))
    y = jax.ops.segment_sum(yb.reshape(n_slots, -1) * slot_w[:, None], slot_tok, num_segments=n + 1)[:n]
    return y.reshape(shp)


def setup_inputs(seed: int = 0) -> dict:
    key = jax.random.key(seed)
    ks = iter(jax.random.split(key, 40))
    n_even = (DEPTH + 1) // 2
    n_odd = DEPTH // 2
    D = D_MODEL

    def nrm(shape, scale):
        return jax.random.normal(next(ks), shape, jnp.float32) * scale

    x = nrm((BATCH, SEQ, D), 1.0)
    c = nrm((BATCH, D), 1.0)
    ctx = nrm((BATCH, CTX_LEN, D), 1.0)
    c_ctx = nrm((D,), 1.0)
    ada_w = nrm((DEPTH, D, 6 * D), 0.5 * D ** -0.5)
    ada_b = nrm((DEPTH, 6 * D), 0.02)
    norm1_g = 1.0 + nrm((DEPTH, D), 0.05)
    norm2_g = 1.0 + nrm((DEPTH, D), 0.05)
    pool_w = nrm((n_even, N_POOL_GROUPS, POOL_GC, POOL_GC), POOL_GC ** -0.5)
    pool_scale = 1.0 + nrm((n_even, D), 0.05)
    ffn_w_gu = nrm((n_even, D, 2 * D_FF), D ** -0.5)
    ffn_w_down = nrm((n_even, D_FF, D), D_FF ** -0.5)
    lru_w_in = nrm((n_odd, D, 2 * D_RNN), D ** -0.5)
    lru_conv_w = nrm((n_odd, CONV_W, D_RNN), CONV_W ** -0.5)
    lru_conv_b = nrm((n_odd, D_RNN), 0.02)
    lru_w_r = nrm((n_odd, 2, LRU_HEADS, LRU_BLOCK, LRU_BLOCK), LRU_BLOCK ** -0.5)
    lru_b_r = nrm((n_odd, 2, D_RNN), 0.02)
    lru_w_i = nrm((n_odd, 2, LRU_HEADS, LRU_BLOCK, LRU_BLOCK), LRU_BLOCK ** -0.5)
    lru_b_i = nrm((n_odd, 2, D_RNN), 0.02)
    a8 = jax.random.uniform(next(ks), (n_odd, 2, D_RNN), jnp.float32, minval=0.9, maxval=0.999)
    a_base = a8 ** (1.0 / LRU_C)
    lru_lambda = jnp.log(a_base) - jnp.log1p(-a_base)
    lru_w_out = nrm((n_odd, D_RNN, D), D_RNN ** -0.5)
    moe_w_router = nrm((n_odd, D, N_EXPERTS), D ** -0.5)
    moe_w_gu = nrm((n_odd, N_EXPERTS, D, 2 * D_EXPERT), D ** -0.5)
    moe_w_down = nrm((n_odd, N_EXPERTS, D_EXPERT, D), D_EXPERT ** -0.5)
    final_g = 1.0 + nrm((D,), 0.05)
    return {"x": x, "c": c, "ctx": ctx, "c_ctx": c_ctx,
            "ada_w": ada_w, "ada_b": ada_b, "norm1_g": norm1_g, "norm2_g": norm2_g,
            "pool_w": pool_w, "pool_scale": pool_scale,
            "ffn_w_gu": ffn_w_gu, "ffn_w_down": ffn_w_down,
            "lru_w_in": lru_w_in, "lru_conv_w": lru_conv_w, "lru_conv_b": lru_conv_b,
            "lru_w_r": lru_w_r, "lru_b_r": lru_b_r, "lru_w_i": lru_w_i, "lru_b_i": lru_b_i,
            "lru_lambda": lru_lambda, "lru_w_out": lru_w_out,
            "moe_w_router": moe_w_router, "moe_w_gu": moe_w_gu, "moe_w_down": moe_w_down,
            "final_g": final_g}


def reference(x, c, ctx, c_ctx, ada_w, ada_b, norm1_g, norm2_g, pool_w, pool_scale,
              ffn_w_gu, ffn_w_down, lru_w_in, lru_conv_w, lru_conv_b, lru_w_r, lru_b_r,
              lru_w_i, lru_b_i, lru_lambda, lru_w_out, moe_w_router, moe_w_gu, moe_w_down,
              final_g):
    bsz, seq_len, d = x.shape
    rows = seq_len // GRID_W
    z = ctx
    for i in range(DEPTH):
        is_last = i == DEPTH - 1
        j = i // N_MIXERS
        mx = (jax.nn.silu(c) @ ada_w[i] + ada_b[i])[:, None, :]
        mz = jax.nn.silu(c_ctx) @ ada_w[i] + ada_b[i]
        sh1x, sc1x, g1x, sh2x, sc2x, g2x = jnp.split(mx, 6, axis=-1)
        sh1z, sc1z, g1z, sh2z, sc2z, g2z = jnp.split(mz, 6, axis=-1)
        hx = modulate(rmsnorm(x, norm1_g[i]), sh1x, sc1x)
        if i % N_MIXERS == 0:
            yx = pool_mixer(hx.reshape(bsz, rows, GRID_W, d), 2, pool_w[j], pool_scale[j])
            x = x + g1x * yx.reshape(bsz, seq_len, d)
            if not is_last:
                hz = modulate(rmsnorm(z, norm1_g[i]), sh1z, sc1z)
                z = z + g1z * pool_mixer(hz, 1, pool_w[j], pool_scale[j])
            x = x + g2x * swiglu(modulate(rmsnorm(x, norm2_g[i]), sh2x, sc2x), ffn_w_gu[j], ffn_w_down[j])
            if not is_last:
                z = z + g2z * swiglu(modulate(rmsnorm(z, norm2_g[i]), sh2z, sc2z), ffn_w_gu[j], ffn_w_down[j])
        else:
            hz = modulate(rmsnorm(z, norm1_g[i]), sh1z, sc1z)
            yx, yz = rglru_mixer(hx, hz, lru_w_in[j], lru_conv_w[j], lru_conv_b[j], lru_w_r[j], lru_b_r[j],
                                 lru_w_i[j], lru_b_i[j], lru_lambda[j], lru_w_out[j], not is_last)
            x = x + g1x * yx
            if not is_last:
                z = z + g1z * yz
            x = x + g2x * moe_swiglu(modulate(rmsnorm(x, norm2_g[i]), sh2x, sc2x),
                                     moe_w_router[j], moe_w_gu[j], moe_w_down[j])
            if not is_last:
                z = z + g2z * moe_swiglu(modulate(rmsnorm(z, norm2_g[i]), sh2z, sc2z),
                                         moe_w_router[j], moe_w_gu[j], moe_w_down[j])
    return rmsnorm(x, final_g)
```

```python
import numpy as np
from contextlib import ExitStack
import concourse.bass as bass
import concourse.mybir as mybir
from concourse.bass_utils import run_bass_kernel_spmd

F32 = mybir.dt.float32
BF16 = mybir.dt.bfloat16
I32 = mybir.dt.int32
ALU = mybir.AluOpType
AF = mybir.ActivationFunctionType
AX = mybir.AxisListType

ENGS = ("pe", "act", "dve", "pool", "sp")
NB = 2
S = 2048
D = 1024
CT = 256
DFF = 2816
DR = 1280
DE = 3584
NE = 8
BLK = 512
NBLK = (NB * S * 2) // BLK + NE
NSLOT = NBLK * BLK
EPS = 1e-6


class Prog:
    def __init__(self):
        self.nc = bass.Bass("TRN2", target_bir_lowering=False)
        self.es = ExitStack()
        self.pes = None
        self.q = {e: [] for e in ENGS}
        self.cnt = {e: 0 for e in ENGS}
        self.esem = {e: self.es.enter_context(self.nc.semaphore("es_" + e)) for e in ENGS}
        self.seen = {e: {} for e in ENGS}
        self.res = {}
        self.rings = {}
        self.dcnt = {}
        self.semobj = {}
        for e in ENGS:
            self.semobj[id(self.esem[e])] = self.esem[e]
        self.n_inst = 0
        self.uid = 0

    def sb(self, name, shape, dt):
        self.uid += 1
        return self.pes.enter_context(self.nc.sbuf_tensor("s%d_%s" % (self.uid, name), list(shape), dt))

    def ps(self, name, shape, dt=F32):
        self.uid += 1
        return self.pes.enter_context(self.nc.psum_tensor("p%d_%s" % (self.uid, name), list(shape), dt))

    def dram(self, name, shape, dt, kind="Internal"):
        return self.nc.dram_tensor(name, list(shape), dt, kind=kind)

    def _st(self, k):
        s = self.res.get(k)
        if s is None:
            s = {"w": None, "r": {}}
            self.res[k] = s
        return s

    def _waits(self, eng, reads, writes):
        need = {}

        def add(tok):
            if tok is None:
                return
            sid, val = tok
            if need.get(sid, 0) < val:
                need[sid] = val

        for r in reads:
            add(self._st(r)["w"])
        for w in writes:
            s = self._st(w)
            add(s["w"])
            for sid, val in s["r"].items():
                add((sid, val))
        own = id(self.esem[eng])
        for sid, val in need.items():
            if eng == "pe" and sid == own:
                continue
            if self.seen[eng].get(sid, 0) >= val:
                continue
            self.seen[eng][sid] = val
            self.q[eng].append(("wait", self.semobj[sid], val))

    def _commit(self, tok, reads, writes):
        sid, val = tok
        for r in reads:
            s = self._st(r)
            if s["r"].get(sid, 0) < val:
                s["r"][sid] = val
        for w in writes:
            s = self._st(w)
            s["w"] = tok
            s["r"] = {}

    def op(self, eng, fn, reads=(), writes=()):
        self._waits(eng, reads, writes)
        self.cnt[eng] += 1
        self.q[eng].append(("inst", fn, self.esem[eng], 1))
        self._commit((id(self.esem[eng]), self.cnt[eng]), reads, writes)
        self.n_inst += 1

    def _ring(self, ring, nring):
        rg = self.rings.get(ring)
        if rg is None:
            rg = {"sems": [self.es.enter_context(self.nc.semaphore("dq_%s_%d" % (ring, i)))
                           for i in range(nring)], "i": 0}
            for s in rg["sems"]:
                self.semobj[id(s)] = s
                self.dcnt[id(s)] = 0
            self.rings[ring] = rg
        sem = rg["sems"][rg["i"] % len(rg["sems"])]
        rg["i"] += 1
        return sem

    def dmafn(self, eng, fn, reads=(), writes=(), ring="d", nring=4):
        sem = self._ring(ring, nring)
        sid = id(sem)
        self._waits(eng, reads, writes)
        if self.dcnt[sid] > 0 and self.seen[eng].get(sid, 0) < self.dcnt[sid]:
            self.seen[eng][sid] = self.dcnt[sid]
            self.q[eng].append(("wait", sem, self.dcnt[sid]))
        self.dcnt[sid] += 16
        self.q[eng].append(("inst", fn, sem, 16))
        self._commit((sid, self.dcnt[sid]), reads, writes)
        self.n_inst += 1

    def dma(self, eng, out, in_, reads=(), writes=(), ring="d", nring=4, **kw):
        self.dmafn(eng, lambda e, o=out, i=in_, k=kw: e.dma_start(out=o, in_=i, **k),
                   reads, writes, ring, nring)

    def gather(self, out, src, idx, reads=(), writes=(), ring="ig", nring=4):
        self.dmafn("pool", lambda e, o=out, s=src, i=idx: e.indirect_dma_start(
            out=o, out_offset=None, in_=s, in_offset=bass.IndirectOffsetOnAxis(ap=i, axis=0)),
            reads, writes, ring, nring)

    def scatter(self, dst, in_, idx, reads=(), writes=(), ring="is", nring=4):
        self.dmafn("pool", lambda e, o=dst, s=in_, i=idx: e.indirect_dma_start(
            out=o, out_offset=bass.IndirectOffsetOnAxis(ap=i, axis=0), in_=s, in_offset=None),
            reads, writes, ring, nring)

    def phase_begin(self):
        self.pes = ExitStack()

    def _emit(self):
        q = self.q

        def replay(lst, e):
            for it in lst:
                if it[0] == "wait":
                    e.wait_ge(it[1], it[2])
                else:
                    it[1](e).then_inc(it[2], it[3])

        with self.nc.Block() as block:
            @block.tensor
            def _(e):
                replay(q["pe"], e)

            @block.scalar
            def _(e):
                replay(q["act"], e)

            @block.vector
            def _(e):
                replay(q["dve"], e)

            @block.gpsimd
            def _(e):
                replay(q["pool"], e)

            @block.sync
            def _(e):
                replay(q["sp"], e)
        self.q = {e: [] for e in ENGS}

    def phase_end(self):
        for e in ENGS:
            for e2 in ENGS:
                if e2 != e and self.cnt[e2] > self.seen[e].get(id(self.esem[e2]), 0):
                    if e == "pe" and e2 == "pe":
                        continue
                    self.seen[e][id(self.esem[e2])] = self.cnt[e2]
                    self.q[e].append(("wait", self.esem[e2], self.cnt[e2]))
            if e != "pe" and self.cnt[e] > self.seen[e].get(id(self.esem[e]), 0):
                self.seen[e][id(self.esem[e])] = self.cnt[e]
                self.q[e].append(("wait", self.esem[e], self.cnt[e]))
            for sid, c in self.dcnt.items():
                if c > self.seen[e].get(sid, 0):
                    self.seen[e][sid] = c
                    self.q[e].append(("wait", self.semobj[sid], c))
        self.res = {}
        self._emit()
        self.pes.close()
        self.pes = None

    def finish(self):
        self.es.close()
        return self.nc


def _pool_mats():
    def amat(n, win):
        A = np.zeros((n, n), np.float64)
        for t in range(n):
            lo = min(max(t - win // 2, 0), n)
            hi = min(max(t - win // 2 + win, 0), n)
            A[t, lo:hi] = 1.0 / (hi - lo)
        return A - np.eye(n)
    atx = np.zeros((128, 4, 128), np.float32)
    atz = np.zeros((128, 4, 2, 256), np.float32)
    for g, win in enumerate((2, 4, 8, 16)):
        a64 = amat(64, win)
        blk = np.zeros((128, 128))
        blk[:64, :64] = a64
        blk[64:, 64:] = a64
        atx[:, g, :] = blk.T
        a256 = amat(256, win).T
        atz[:, g, 0, :] = a256[:128]
        atz[:, g, 1, :] = a256[128:]
    return atx, atz


def build(upto=9, debug=False):
    P = Prog()
    nc = P.nc
    dr = P.dram
    x_in = dr("x", [NB * S, D], F32, kind="ExternalInput")
    z_in = dr("ctx", [NB * CT, D], F32, kind="ExternalInput")
    cT_in = dr("cT", [128, 8, 3], F32, kind="ExternalInput")
    ada_w = dr("ada_w", [2, D, 6 * D], F32, kind="ExternalInput")
    ada_b = dr("ada_b", [2, 6 * D], F32, kind="ExternalInput")
    n1g = dr("norm1_g", [2, D], F32, kind="ExternalInput")
    n2g = dr("norm2_g", [2, D], F32, kind="ExternalInput")
    pool_w = dr("pool_w", [4, 256, 256], F32, kind="ExternalInput")
    pool_scale = dr("pool_scale", [D], F32, kind="ExternalInput")
    w_gu = dr("ffn_w_gu", [128, 11, 2 * 8 * 256], F32, kind="ExternalInput")
    w_dn = dr("ffn_w_down", [DFF, D], F32, kind="ExternalInput")
    atx_in = dr("atx", [128, 4, 128], F32, kind="ExternalInput")
    atz_in = dr("atz", [128, 4, 2, 256], F32, kind="ExternalInput")
    if upto >= 2:
        lru_w_in = dr("lru_w_in", [D, 2 * DR], F32, kind="ExternalInput")
        lru_pp = dr("lru_pp", [128, 10, 12], F32, kind="ExternalInput")
        lru_w_r = dr("lru_w_r", [2, 10, 128, 128], F32, kind="ExternalInput")
        lru_w_i = dr("lru_w_i", [2, 10, 128, 128], F32, kind="ExternalInput")
        lru_w_out = dr("lru_w_out", [DR, D], F32, kind="ExternalInput")
    if upto >= 3:
        mconst = dr("mconst", [128, 632], F32, kind="ExternalInput")
        w_router = dr("moe_w_router", [D, NE], F32, kind="ExternalInput")
        moe_gu = dr("moe_w_gu", [NE * 128 * 8, 4 * 2 * 896], F32, kind="ExternalInput")
        moe_dn = dr("moe_w_down", [NE * 128 * 4, 7 * D], F32, kind="ExternalInput")
    final_g = dr("final_g", [D], F32, kind="ExternalInput")
    out_h = dr("out", [NB * S, D], F32, kind="ExternalOutput")
    x_s = dr("x_s", [NB * S, D], F32)
    z_s = dr("z_s", [NB * CT, D], F32)
    modrows = dr("modrows", [3, 2, 6 * D], F32)
    dbg = {}

    P.phase_begin()
    cT = P.sb("cT", [128, 8, 3], F32)
    scT = P.sb("scT", [128, 8, 3], F32)
    rows = P.sb("rows", [3, 2, 6 * D], F32)
    adab = P.sb("adab", [3, 2, 6 * D], F32)
    gbc = P.sb("gbc", [3, 5, D], F32)
    wts = [P.sb("adaw%d" % i, [128, 8, 512], F32) for i in range(2)]
    pa = [P.ps("pa%d" % i, [128, 512], F32) for i in range(2)]
    P.dma("sp", cT[:], cT_in.ap(), writes=["cT"], ring="ld")
    for l in range(2):
        P.dma("act", adab[:, l, :], ada_b.ap()[l].partition_broadcast(3), writes=[("adab", l)], ring="ld")
    for i, src in enumerate((n1g.ap()[0], n1g.ap()[1], n2g.ap()[0], n2g.ap()[1], pool_scale.ap())):
        P.dma("act", gbc[:, i, :], src.partition_broadcast(3), writes=[("gbc", i)], ring="ld")
    P.op("act", lambda e: e.activation(scT[:], cT[:], AF.Silu), reads=["cT"], writes=["scT"])
    n = 0
    for l in range(2):
        for ft in range(12):
            b = n % 2
            n += 1
            P.dma("sp", wts[b][:], ada_w.ap()[l][:, ft * 512:(ft + 1) * 512].rearrange("(c p) f -> p c f", p=128),
                  writes=[("adaw", b)], ring="adaw", nring=2)
            for kc in range(8):
                P.op("pe", lambda e, b=b, kc=kc: e.matmul(pa[b][0:3, :], scT[:, kc, :], wts[b][:, kc, :],
                                                           start=(kc == 0), stop=(kc == 7)),
                     reads=["scT", ("adaw", b)], writes=[("pa", b)])
            P.op("dve", lambda e, b=b, l=l, ft=ft: e.tensor_tensor(
                rows[:, l, ft * 512:(ft + 1) * 512], pa[b][0:3, :], adab[:, l, ft * 512:(ft + 1) * 512], ALU.add),
                reads=[("pa", b), ("adab", l)], writes=[("rows", l, ft)])
    for l in range(2):
        allr = [("rows", l, ft) for ft in range(12)]
        P.op("dve", lambda e, l=l: e.scalar_tensor_tensor(rows[:, l, D:2 * D], rows[:, l, D:2 * D], 1.0, gbc[:, l, :], ALU.add, ALU.mult),
             reads=allr + [("gbc", l)], writes=allr)
        P.op("dve", lambda e, l=l: e.scalar_tensor_tensor(rows[:, l, 4 * D:5 * D], rows[:, l, 4 * D:5 * D], 1.0, gbc[:, 2 + l, :], ALU.add, ALU.mult),
             reads=allr + [("gbc", 2 + l)], writes=allr)
    allr0 = [("rows", 0, ft) for ft in range(12)]
    P.op("dve", lambda e: e.tensor_tensor(rows[:, 0, 2 * D:3 * D], rows[:, 0, 2 * D:3 * D], gbc[:, 4, :], ALU.mult),
         reads=allr0 + [("gbc", 4)], writes=allr0)
    P.dma("sp", modrows.ap(), rows[:], reads=[("rows", l, ft) for l in range(2) for ft in range(12)], writes=["modrows"], ring="st")
    P.phase_end()

    def load_bc(tile, stream, layer, reads_key="modrows"):
        for j in range(6):
            P.dma("act" if j % 2 else "sp", tile[:, j, :], modrows.ap()[stream, layer, j * D:(j + 1) * D].partition_broadcast(128),
                  writes=[("bc", j)], ring="ldbc", nring=6)

    def norm_mod(xt_ap, out_ap, bc, jg, jsh, sq, ssum, rstd, tmp, keys_r, key_w, tag, bcname="bc"):
        P.op("act", lambda e: e.activation(sq[:], xt_ap, AF.Square, accum_out=ssum[:, 0:1]),
             reads=keys_r, writes=[("ssum", tag), "sq"])
        P.op("act", lambda e: e.activation(rstd[:, 0:1], ssum[:, 0:1], AF.Sqrt, bias=EPS, scale=1.0 / D),
             reads=[("ssum", tag)], writes=[("rstd", tag)])
        P.op("dve", lambda e: e.reciprocal(rstd[:, 0:1], rstd[:, 0:1]), reads=[("rstd", tag)], writes=[("rstd", tag)])
        P.op("dve", lambda e: e.scalar_tensor_tensor(tmp[:], xt_ap, rstd[:, 0:1], bc[:, jg, :], ALU.mult, ALU.mult),
             reads=keys_r + [("rstd", tag), (bcname, jg)], writes=[("tmp", tag)])
        P.op("dve", lambda e: e.tensor_tensor(out_ap, tmp[:], bc[:, jsh, :], ALU.add),
             reads=[("tmp", tag), (bcname, jsh)], writes=[key_w])

    if upto >= 1:
        P.phase_begin()
        wdn = P.sb("wdn", [128, 22, D], BF16)
        pw = P.sb("pw", [128, 4, 2, 256], BF16)
        atx = P.sb("atx", [128, 4, 128], BF16)
        atz = P.sb("atz", [128, 4, 2, 256], BF16)
        identf = P.sb("identf", [128, 128], F32)
        ident = P.sb("ident", [128, 128], BF16)
        bcp = P.sb("bcp", [128, 5, D], F32)
        g2bc = [P.sb("g2bc%d" % i, [128, D], F32) for i in range(2)]
        xt2 = [P.sb("xt%d" % i, [128, 4, D], F32) for i in range(2)]
        hm = P.sb("hm", [128, 4, D], BF16)
        sq = P.sb("sq", [128, D], BF16)
        tmps = [P.sb("tmp%d" % i, [128, D], F32) for i in range(2)]
        ssum4 = P.sb("ssum4", [128, 4], F32)
        rstd4 = P.sb("rstd4", [128, 4], F32)
        ppT = P.sb("ppT", [128, 8, 128], BF16)
        hT2 = [P.sb("hT%d" % i, [128, 8, 512], BF16) for i in range(2)]
        act = P.sb("act", [128, 22, 512], BF16)
        sg = [P.sb("sg%d" % i, [128, 512], BF16) for i in range(2)]
        upds = [P.sb("upd%d" % i, [128, 512], F32) for i in range(2)]
        wgs = [P.sb("wgs%d" % i, [128, 2, 8, 256], BF16) for i in range(2)]
        pp = [P.ps("pp%d" % i, [128, 512], F32) for i in range(4)]
        py = [P.ps("py%d" % i, [128, 512], F32) for i in range(2)]
        pa0 = P.ps("pa0", [128, 512], F32)
        ptr = P.ps("ptr", [128, 8, 128], BF16)

        P.dma("pool", atz[:], atz_in.ap(), writes=["atz"], ring="ldw")
        P.dma("pool", atx[:], atx_in.ap(), writes=["atx"], ring="ldw")
        P.dma("pool", pw[:], pool_w.ap().rearrange("g (k p) n -> p g k n", p=128), writes=["pw"], ring="ldw")
        P.dma("pool", wdn[:], w_dn.ap().rearrange("(c p) n -> p c n", p=128), writes=["wdn"], ring="ldw")
        P.op("pool", lambda e: e.memset(identf[:], 0.0), writes=["identf"])
        P.op("pool", lambda e: e.affine_select(identf[:], identf[:], [[-1, 128]], ALU.not_equal, 1.0, base=0, channel_multiplier=1),
             reads=["identf"], writes=["identf"])
        P.op("dve", lambda e: e.tensor_copy(ident[:], identf[:]), reads=["identf"], writes=["ident"])

        groups = [(2, z_in, z_s, 0, True)]
        for b in range(NB):
            for gi in range(4):
                groups.append((b, x_in, x_s, b * S + gi * 512, False))
        NG = len(groups)
        BCJ = (0, 1, 2, 3, 4)
        xk = lambda gp, t: [("xt", gp, t), ("xt", gp, t, 0), ("xt", gp, t, 1)]

        def prep_steps(g):
            stream, src, dst, r0, is_ctx = groups[g]
            gp = g % 2
            xt = xt2[gp]
            hT = hT2[gp]
            new_stream = (g == 0) or (groups[g - 1][0] != stream)
            st = {}

            def m0():
                if new_stream:
                    for i, j in enumerate(BCJ):
                        P.dma("act" if i % 2 else "sp", bcp[:, i, :], modrows.ap()[stream, 0, j * D:(j + 1) * D].partition_broadcast(128),
                              writes=[("bcp", i)], ring="ldbc", nring=6)
                P.dma("act", g2bc[gp][:], modrows.ap()[stream, 0, 5 * D:6 * D].partition_broadcast(128), writes=[("g2bc", gp)], ring="ldbc", nring=6)
                P.dma("sp", xt[:], src.ap()[r0:r0 + 512, :].rearrange("(t p) d -> p t d", p=128),
                      writes=[k for t in range(4) for k in xk(gp, t)], ring="ldx", nring=2)

            def nrm4(jg, jsh, tag):
                for t in range(4):
                    P.op("act", lambda e, t=t: e.activation(sq[:], xt[:, t, :], AF.Square, accum_out=ssum4[:, t:t + 1]),
                         reads=xk(gp, t), writes=[("ssum4", t), "sq"])
                P.op("act", lambda e: e.activation(rstd4[:], ssum4[:], AF.Sqrt, bias=EPS, scale=1.0 / D),
                     reads=[("ssum4", t) for t in range(4)], writes=["rstd4"])
                P.op("dve", lambda e: e.reciprocal(rstd4[:], rstd4[:]), reads=["rstd4"], writes=["rstd4"])
                for t in range(4):
                    i2 = t % 2
                    P.op("dve", lambda e, t=t, i2=i2: e.scalar_tensor_tensor(tmps[i2][:], xt[:, t, :], rstd4[:, t:t + 1], bcp[:, jg, :], ALU.mult, ALU.mult),
                         reads=xk(gp, t) + ["rstd4", ("bcp", jg)], writes=[("tmp", i2)])
                    P.op("dve", lambda e, t=t, i2=i2: e.tensor_tensor(hm[:, t, :], tmps[i2][:], bcp[:, jsh, :], ALU.add),
                         reads=[("tmp", i2), ("bcp", jsh)], writes=[("hm", t)])

            def A(t):
                for cc in range(8):
                    gq = cc // 2
                    dstp = pa0 if cc < 4 else py[1]
                    dk = "pa0" if cc < 4 else ("py", 1)
                    o = dstp[:, (cc % 4) * 128:(cc % 4 + 1) * 128]
                    if not is_ctx:
                        P.op("pe", lambda e, o=o, t=t, cc=cc, gq=gq: e.matmul(o, hm[:, t, cc * 128:(cc + 1) * 128], atx[:, gq, :], start=True, stop=True),
                             reads=[("hm", t), "atx"], writes=[dk])
                    else:
                        zb, to = t // 2, t % 2
                        for ki in range(2):
                            P.op("pe", lambda e, o=o, zb=zb, ki=ki, cc=cc, gq=gq, to=to: e.matmul(
                                o, hm[:, zb * 2 + ki, cc * 128:(cc + 1) * 128], atz[:, gq, ki, to * 128:(to + 1) * 128],
                                start=(ki == 0), stop=(ki == 1)),
                                reads=[("hm", zb * 2 + ki), "atz"], writes=[dk])

            def C(t):
                P.op("act", lambda e: e.activation(ppT[:, 0:4, :].rearrange("p a b -> p (a b)"), pa0[:], AF.Copy), reads=["pa0"], writes=[("ppT", 0)])
                P.op("act", lambda e: e.activation(ppT[:, 4:8, :].rearrange("p a b -> p (a b)"), py[1][:], AF.Copy), reads=[("py", 1)], writes=[("ppT", 1)])

            def B(t):
                for gq in range(4):
                    dstp = py[0] if gq < 2 else pa0
                    dk = ("py", 0) if gq < 2 else "pa0"
                    for kc in range(2):
                        P.op("pe", lambda e, gq=gq, kc=kc, dstp=dstp: e.matmul(dstp[:, (gq % 2) * 256:(gq % 2 + 1) * 256], ppT[:, gq * 2 + kc, :], pw[:, gq, kc, :],
                                                                           start=(kc == 0), stop=(kc == 1)),
                             reads=[("ppT", gq // 2), "pw"], writes=[dk])

            def Ustep(t):
                for h in range(2):
                    srcp = py[0] if h == 0 else pa0
                    sk = ("py", 0) if h == 0 else "pa0"
                    P.op("dve", lambda e, h=h, srcp=srcp: e.tensor_tensor(upds[h][:], srcp[:], bcp[:, 2, h * 512:(h + 1) * 512], ALU.mult),
                         reads=[sk, ("bcp", 2)], writes=[("upd", h)])
                    P.op("dve", lambda e, h=h, t=t: e.tensor_tensor(xt[:, t, h * 512:(h + 1) * 512], xt[:, t, h * 512:(h + 1) * 512], upds[h][:], ALU.add),
                         reads=[("upd", h), ("xt", gp, t, h)], writes=[("xt", gp, t, h)])

            def T(t):
                for c in range(8):
                    P.op("pe", lambda e, t=t, c=c: e.transpose(ptr[:, c, :], hm[:, t, c * 128:(c + 1) * 128], ident[:]),
                         reads=[("hm", t), "ident"], writes=["ptr"])
                P.op("act", lambda e, t=t: e.activation(hT[:, :, t * 128:(t + 1) * 128], ptr[:], AF.Copy),
                     reads=["ptr"], writes=[("hT", gp, t)])

            sl = {i: [] for i in range(22)}
            sl[0] = [m0]
            sl[5] = [lambda: nrm4(1, 0, 0)]
            for t in range(4):
                sl[7 + 2 * t].append(lambda t=t: A(t))
                sl[8 + 2 * t].append(lambda t=t: C(t))
                sl[8 + 2 * t].append(lambda t=t: B(t))
                sl[8 + 2 * t].append(lambda t=t: Ustep(t))
            sl[15].append(lambda: nrm4(4, 3, 1))
            sl[20] += [lambda: T(0), lambda: T(1)]
            sl[21] += [lambda: T(2), lambda: T(3)]
            return sl

        wg_state = {"n": 0}

        def gateup_tile(g, ft):
            gp = g % 2
            hT = hT2[gp]
            hTk = [("hT", gp, t) for t in range(4)]
            if ft % 2 == 0:
                wb = wg_state["n"] % 2
                wg_state["n"] += 1
                wg_state["wb"] = wb
                P.dma("pool", wgs[wb][:].rearrange("p h c f -> p (h c f)"), w_gu.ap()[:, ft // 2, :],
                      writes=[("wgs", wb, 0), ("wgs", wb, 1)], ring="ldwg", nring=2)
            wb = wg_state["wb"]
            fi = ft % 2
            pb = (ft % 2) * 2
            for half in range(2):
                for kc in range(8):
                    P.op("pe", lambda e, wb=wb, half=half, kc=kc, fi=fi, pb=pb: e.matmul(
                        pp[pb + half][:], wgs[wb][:, half, kc, fi * 128:(fi + 1) * 128], hT[:, kc, :],
                        start=(kc == 0), stop=(kc == 7)),
                        reads=hTk + [("wgs", wb, half)], writes=[("pp", pb + half)])
            sgi = ft % 2
            P.op("act", lambda e, pb=pb, sgi=sgi: e.activation(sg[sgi][:], pp[pb][:], AF.Silu),
                 reads=[("pp", pb)], writes=[("sg", sgi)])
            P.op("dve", lambda e, pb=pb, sgi=sgi, ft=ft: e.tensor_tensor(act[:, ft, :], sg[sgi][:], pp[pb + 1][:], ALU.mult),
                 reads=[("sg", sgi), ("pp", pb + 1)], writes=[("act", ft)])

        def down(g):
            stream, src, dst, r0, is_ctx = groups[g]
            gp = g % 2
            xt = xt2[gp]
            actk = [("act", ft) for ft in range(22)]
            for t in range(4):
                for h in range(2):
                    for ft in range(22):
                        P.op("pe", lambda e, t=t, h=h, ft=ft: e.matmul(py[h][:], act[:, ft, t * 128:(t + 1) * 128], wdn[:, ft, h * 512:(h + 1) * 512],
                                                                       start=(ft == 0), stop=(ft == 21)),
                             reads=actk + ["wdn"], writes=[("py", h)])
                    P.op("dve", lambda e, h=h: e.tensor_tensor(upds[h][:], py[h][:], g2bc[gp][:, h * 512:(h + 1) * 512], ALU.mult),
                         reads=[("py", h), ("g2bc", gp)], writes=[("upd", h)])
                    P.op("dve", lambda e, h=h, t=t: e.tensor_tensor(xt[:, t, h * 512:(h + 1) * 512], xt[:, t, h * 512:(h + 1) * 512], upds[h][:], ALU.add),
                         reads=[("upd", h), ("xt", gp, t, h)], writes=[("xt", gp, t, h)])
            P.dma("sp", dst.ap()[r0:r0 + 512, :].rearrange("(t p) d -> p t d", p=128), xt[:],
                  reads=[k for t in range(4) for k in xk(gp, t)], writes=[("xs", id(dst), r0)], ring="stx", nring=2)

        sl0 = prep_steps(0)
        for i in range(22):
            for f_ in sl0[i]:
                f_()
        for g in range(NG):
            sln = prep_steps(g + 1) if g + 1 < NG else None
            for ft in range(22):
                gateup_tile(g, ft)
                if sln is not None:
                    for f_ in sln[ft]:
                        f_()
            down(g)
        P.phase_end()

    if upto >= 2:
        s_s = dr("s_s", [NB, 10, 128, S], BF16)
        TT = CT + S
        UP = 2 + CT + 1 + 2 + S + 1
        P.phase_begin()
        lpp = P.sb("lpp", [128, 10, 12], F32)
        hb = P.sb("hb", [128, 10, 4], F32)
        hcn = P.sb("hcn", [128, 10, 2], F32)
        hncn = P.sb("hncn", [128, 10, 2], F32)
        tl = [P.sb("tl%d" % i, [128, 10, 2], F32) for i in range(5)]
        wr = P.sb("wr", [128, 2, 10, 128], BF16)
        wi = P.sb("wi", [128, 2, 10, 128], BF16)
        win_u = [P.sb("winu%d" % i, [128, 8, 128], BF16) for i in range(2)]
        win_g = [P.sb("wing%d" % i, [128, 8, 128], BF16) for i in range(3)]
        identf = P.sb("identf", [128, 128], F32)
        ident = P.sb("ident", [128, 128], BF16)
        ssum = [P.sb("ssum%d" % i, [128, 1], F32) for i in range(4)]
        rstd = [P.sb("rstd%d" % i, [128, 1], F32) for i in range(4)]
        hT = P.sb("hT", [128, 8, TT], BF16)
        u_raw = [P.sb("u_raw%d" % i, [128, UP], F32) for i in range(2)]
        uc = [P.sb("uc%d" % i, [128, TT], F32) for i in range(2)]
        ucb = [P.sb("ucb%d" % i, [128, TT], BF16) for i in range(2)]
        gg = P.sb("gg", [128, S], BF16)
        Rb = [P.sb("Rb%d" % i, [128, TT], F32) for i in range(2)]
        Ib = [P.sb("Ib%d" % i, [128, TT], F32) for i in range(2)]
        Ab = [P.sb("Ab%d" % i, [128, TT], F32) for i in range(2)]
        Tb = [P.sb("Tb%d" % i, [128, TT], F32) for i in range(2)]
        Hb = [P.sb("Hb%d" % i, [128, TT], F32) for i in range(2)]
        sbf = P.sb("sbf", [128, S], BF16)
        pu = [P.ps("pu%d" % i, [128, 512], F32) for i in range(2)]
        pr = [P.ps("pr%d" % i, [128, 512], F32) for i in range(2)]
        pi = [P.ps("pi%d" % i, [128, 512], F32) for i in range(2)]
        ptr = [P.ps("ptr%d" % i, [128, 8, 128], BF16) for i in range(2)]
        K5 = lambda nm, d: [(nm, d, j) for j in range(5)]
        xts = [Rb[0][:, 0:D], Rb[1][:, 0:D], Tb[0][:, 0:D], Tb[1][:, 0:D]]
        xts_k = [K5("R", 0), K5("R", 1), K5("T", 0), K5("T", 1)]
        tmp = [Ib[i][:, 0:D] for i in range(2)]
        tmp_k = [K5("I", i) for i in range(2)]
        hm1 = [ucb[i][:, 0:D] for i in range(2)]
        hm1_k = [[("ucb", i, j) for j in range(5)] for i in range(2)]
        sq = gg[:, 0:D]
        sq_k = [("gg", j) for j in range(4)]
        bcz = Ab[0][:, 0:2 * D].rearrange("p (a b) -> p a b", a=2)
        bcz_k = K5("A", 0)
        bcx = Ab[1][:, 0:2 * D].rearrange("p (a b) -> p a b", a=2)
        bcx_k = K5("A", 1)

        P.dma("sp", lpp[:], lru_pp.ap(), writes=["lpp"], ring="ld")
        P.dma("pool", wr[:], lru_w_r.ap().rearrange("d h c n -> c d h n"), writes=["wr"], ring="ldw")
        P.dma("pool", wi[:], lru_w_i.ap().rearrange("d h c n -> c d h n"), writes=["wi"], ring="ldw")
        P.op("pool", lambda e: e.memset(identf[:], 0.0), writes=["identf"])
        P.op("pool", lambda e: e.affine_select(identf[:], identf[:], [[-1, 128]], ALU.not_equal, 1.0, base=0, channel_multiplier=1),
             reads=["identf"], writes=["identf"])
        P.op("dve", lambda e: e.tensor_copy(ident[:], identf[:]), reads=["identf"], writes=["ident"])
        for i in range(2):
            P.op("pool", lambda e, i=i: e.memset(u_raw[i][:], 0.0), writes=[("u_pad", i)])
        lam = lpp[:, :, 9:11]
        t0, t1, t2, t3, t4 = [t[:] for t in tl]
        P.op("act", lambda e: e.activation(t0, lam, AF.Abs), reads=["lpp"], writes=["t0"])
        P.op("act", lambda e: e.activation(t0, t0, AF.Exp, scale=-1.0), reads=["t0"], writes=["t0"])
        P.op("dve", lambda e: e.tensor_scalar(t1, t0, 2.0, None, ALU.add), reads=["t0"], writes=["t1"])
        P.op("dve", lambda e: e.reciprocal(t1, t1), reads=["t1"], writes=["t1"])
        P.op("dve", lambda e: e.tensor_tensor(t1, t1, t0, ALU.mult), reads=["t1", "t0"], writes=["t1"])
        P.op("dve", lambda e: e.tensor_tensor(t2, t1, t1, ALU.mult), reads=["t1"], writes=["t2"])
        P.op("dve", lambda e: e.tensor_scalar(t3, t2, 1.0 / 9.0, None, ALU.mult), reads=["t2"], writes=["t3"])
        for cst in (1.0 / 7.0, 1.0 / 5.0, 1.0 / 3.0):
            P.op("dve", lambda e, cst=cst: e.scalar_tensor_tensor(t3, t3, cst, t2, ALU.add, ALU.mult), reads=["t3", "t2"], writes=["t3"])
        P.op("dve", lambda e: e.scalar_tensor_tensor(t3, t3, 1.0, t1, ALU.add, ALU.mult), reads=["t3", "t1"], writes=["t3"])
        P.op("dve", lambda e: e.tensor_scalar(t4, lam, -1.0, 0.0, ALU.mult, ALU.max), reads=["lpp"], writes=["t4"])
        P.op("dve", lambda e: e.scalar_tensor_tensor(t4, t3, 2.0, t4, ALU.mult, ALU.add), reads=["t3", "t4"], writes=["t4"])
        P.op("dve", lambda e: e.tensor_scalar(hcn[:], t4, -4.0, None, ALU.mult), reads=["t4"], writes=["hcn"])
        P.op("dve", lambda e: e.tensor_scalar(hncn[:], t4, 4.0, None, ALU.mult), reads=["t4"], writes=["hncn"])
        P.op("dve", lambda e: e.tensor_scalar(hb[:], lpp[:, :, 5:9], 0.5, None, ALU.mult), reads=["lpp"], writes=["hb"])

        tiles = [(0, CT)] + [(CT + j * 512, 512) for j in range(4)]
        ucol = [2] + [(2 + CT + 1 + 2) + j * 512 for j in range(4)]
        hTk = [("hT", c) for c in range(0, TT, 128)]
        order = {0: [0, 1, 2, 3, 4], 1: [0, 4, 3, 2, 1]}
        state = {"xn": 0, "pn": 0}

        def build_hT(b):
            for j in range(2):
                P.dma("sp" if j == 0 else "act", bcz[:, j, :], modrows.ap()[2, 1, j * D:(j + 1) * D].partition_broadcast(128),
                      writes=bcz_k, ring="ldbc", nring=6)
                P.dma("act" if j == 0 else "sp", bcx[:, j, :], modrows.ap()[b, 1, j * D:(j + 1) * D].partition_broadcast(128),
                      writes=bcx_k, ring="ldbc", nring=6)
            srcs = [(z_s, b * CT + t * 128, bcz, bcz_k, t * 128) for t in range(2)] + \
                   [(x_s, b * S + t * 128, bcx, bcx_k, CT + t * 128) for t in range(16)]
            def front(src, r0, bct, bk, col0):
                xb = state["xn"] % 4
                hb_ = state["xn"] % 2
                state["xn"] += 1
                P.dma("sp" if xb % 2 == 0 else "act", xts[xb], src.ap()[r0:r0 + 128, :], writes=xts_k[xb], ring="ldx4", nring=4)
                P.op("act", lambda e, xb=xb: e.activation(sq, xts[xb], AF.Square, accum_out=ssum[xb][:, 0:1]),
                     reads=xts_k[xb], writes=sq_k + [("ssum", xb)])
                P.op("act", lambda e, xb=xb: e.activation(rstd[xb][:, 0:1], ssum[xb][:, 0:1], AF.Sqrt, bias=EPS, scale=1.0 / D),
                     reads=[("ssum", xb)], writes=[("rstd", xb)])
                P.op("dve", lambda e, xb=xb: e.reciprocal(rstd[xb][:, 0:1], rstd[xb][:, 0:1]), reads=[("rstd", xb)], writes=[("rstd", xb)])
                P.op("dve", lambda e, xb=xb, hb_=hb_, bct=bct: e.scalar_tensor_tensor(tmp[hb_], xts[xb], rstd[xb][:, 0:1], bct[:, 1, :], ALU.mult, ALU.mult),
                     reads=xts_k[xb] + [("rstd", xb)] + bk, writes=tmp_k[hb_])
                P.op("dve", lambda e, hb_=hb_, bct=bct: e.tensor_tensor(hm1[hb_], tmp[hb_], bct[:, 0, :], ALU.add),
                     reads=tmp_k[hb_] + bk, writes=hm1_k[hb_] + [("hm1", hb_)])
                for c in range(8):
                    P.op("pe", lambda e, c=c, hb_=hb_: e.transpose(ptr[hb_][:, c, :], hm1[hb_][:, c * 128:(c + 1) * 128], ident[:]),
                         reads=[("hm1", hb_), "ident"], writes=[("ptr", hb_)])
                return hb_

            def back(col0, hb_):
                P.op("act", lambda e, col0=col0, hb_=hb_: e.activation(hT[:, :, col0:col0 + 128], ptr[hb_][:], AF.Copy),
                     reads=[("ptr", hb_)], writes=[("hT", col0)])

            pend = None
            for (src, r0, bct, bk, col0) in srcs:
                hb_ = front(src, r0, bct, bk, col0)
                if pend is not None:
                    back(*pend)
                pend = (col0, hb_)
            back(*pend)

        def a1(b, h, par):
            wb = par
            gb = (b * 10 + h) % 3
            P.dma("pool", win_g[gb][:], lru_w_in.ap()[:, h * 128:(h + 1) * 128].rearrange("(c p) f -> p c f", p=128),
                  writes=[("wing", gb)], ring="ldwin0", nring=2)
            P.dma("pool", win_u[wb][:], lru_w_in.ap()[:, DR + h * 128:DR + (h + 1) * 128].rearrange("(c p) f -> p c f", p=128),
                  writes=[("winu", wb)], ring="ldwin1", nring=2)
            for j, (c0, n) in enumerate(tiles):
                pb = j % 2
                for kc in range(8):
                    P.op("pe", lambda e, pb=pb, kc=kc, c0=c0, n=n, wb=wb: e.matmul(pu[pb][:, 0:n], win_u[wb][:, kc, :], hT[:, kc, c0:c0 + n],
                                                                              start=(kc == 0), stop=(kc == 7)),
                         reads=hTk + [("winu", wb)], writes=[("pu", pb)])
                P.op("dve", lambda e, pb=pb, n=n, j=j, par=par: e.tensor_copy(u_raw[par][:, ucol[j]:ucol[j] + n], pu[pb][:, 0:n]),
                     reads=[("pu", pb), ("u_pad", par)], writes=[("u_raw", par, j)])

        def a2(b, h, par):
            for j, (c0, n) in enumerate(tiles):
                nb_ = [j] if j == 0 else [jj for jj in (j - 1, j, j + 1) if 1 <= jj <= 4]
                urk = [("u_raw", par, jj) for jj in nb_]
                r0 = ucol[j]
                P.op("act", lambda e, c0=c0, n=n, r0=r0, h=h, par=par: e.activation(uc[par][:, c0:c0 + n], u_raw[par][:, r0:r0 + n], AF.Identity,
                                                                                 bias=lpp[:, h, 4:5], scale=lpp[:, h, 2:3]),
                     reads=urk + ["lpp"], writes=[("uc", par, j)])

        def a3(b, h, par):
            for j, (c0, n) in enumerate(tiles):
                nb_ = [j] if j == 0 else [jj for jj in (j - 1, j, j + 1) if 1 <= jj <= 4]
                urk = [("u_raw", par, jj) for jj in nb_]
                r0 = ucol[j]
                for k in (0, 1, 3):
                    P.op("dve", lambda e, c0=c0, n=n, r0=r0, h=h, k=k, par=par: e.scalar_tensor_tensor(
                        uc[par][:, c0:c0 + n], u_raw[par][:, r0 - 2 + k:r0 - 2 + k + n], lpp[:, h, k:k + 1], uc[par][:, c0:c0 + n], ALU.mult, ALU.add),
                        reads=urk + ["lpp", ("uc", par, j)], writes=[("uc", par, j)])

        def a4(b, h, par):
            for j, (c0, n) in enumerate(tiles):
                P.op("act", lambda e, c0=c0, n=n, par=par: e.activation(ucb[par][:, c0:c0 + n], uc[par][:, c0:c0 + n], AF.Copy),
                     reads=[("uc", par, j)], writes=[("ucb", par, j)])

        def b1(b, h, par):
            for d in range(2):
                for j in order[d]:
                    c0, n = tiles[j]
                    pb = state["pn"] % 2
                    state["pn"] += 1
                    P.op("pe", lambda e, pb=pb, c0=c0, n=n, d=d: e.matmul(pr[pb][:, 0:n], wr[:, d, h, :], ucb[par][:, c0:c0 + n], start=True, stop=True),
                         reads=[("ucb", par, j), "wr"], writes=[("pr", pb)])
                    P.op("pe", lambda e, pb=pb, c0=c0, n=n, d=d: e.matmul(pi[pb][:, 0:n], wi[:, d, h, :], ucb[par][:, c0:c0 + n], start=True, stop=True),
                         reads=[("ucb", par, j), "wi"], writes=[("pi", pb)])
                    P.op("act", lambda e, pb=pb, c0=c0, n=n, d=d: e.activation(Rb[d][:, c0:c0 + n], pr[pb][:, 0:n], AF.Tanh,
                                                                          bias=hb[:, h, d:d + 1], scale=0.5),
                         reads=[("pr", pb), "hb"], writes=[("R", d, j)])
                    P.op("act", lambda e, pb=pb, c0=c0, n=n, d=d: e.activation(Ib[d][:, c0:c0 + n], pi[pb][:, 0:n], AF.Tanh,
                                                                          bias=hb[:, h, 2 + d:3 + d], scale=0.5),
                         reads=[("pi", pb), "hb"], writes=[("I", d, j)])

        def b2(b, h, par):
            for d in range(2):
                for j in order[d]:
                    c0, n = tiles[j]
                    sl = slice(c0, c0 + n)
                    P.op("act", lambda e, d=d, sl=sl: e.activation(Ab[d][:, sl], Rb[d][:, sl], AF.Exp, bias=hcn[:, h, d:d + 1], scale=hcn[:, h, d:d + 1]),
                         reads=[("R", d, j), "hcn"], writes=[("A", d, j)])
                    P.op("act", lambda e, d=d, sl=sl: e.activation(Tb[d][:, sl], Rb[d][:, sl], AF.Tanh, bias=hncn[:, h, d:d + 1], scale=hncn[:, h, d:d + 1]),
                         reads=[("R", d, j), "hncn"], writes=[("T", d, j)])
                    P.op("act", lambda e, d=d, sl=sl: e.activation(Rb[d][:, sl], Ab[d][:, sl], AF.Square),
                         reads=[("A", d, j)], writes=[("R", d, j)])
                    P.op("dve", lambda e, d=d, sl=sl: e.scalar_tensor_tensor(Tb[d][:, sl], Rb[d][:, sl], 1.0, Tb[d][:, sl], ALU.add, ALU.mult),
                         reads=[("R", d, j), ("T", d, j)], writes=[("T", d, j)])
                    P.op("dve", lambda e, d=d, sl=sl: e.scalar_tensor_tensor(Ib[d][:, sl], Ib[d][:, sl], 1.0, uc[par][:, sl], ALU.add, ALU.mult),
                         reads=[("I", d, j), ("uc", par, j)], writes=[("I", d, j)])

        def b3a(b, h, par):
            for d in range(2):
                for j in order[d]:
                    c0, n = tiles[j]
                    sl = slice(c0, c0 + n)
                    P.op("act", lambda e, d=d, sl=sl: e.activation(Tb[d][:, sl], Tb[d][:, sl], AF.Sqrt, scale=0.25),
                         reads=[("T", d, j)], writes=[("T", d, j)])

        def b3b(b, h, par):
            for d in range(2):
                prev = None
                for j in order[d]:
                    c0, n = tiles[j]
                    sl = slice(c0, c0 + n)
                    P.op("dve", lambda e, d=d, sl=sl: e.tensor_tensor(Tb[d][:, sl], Tb[d][:, sl], Ib[d][:, sl], ALU.mult),
                         reads=[("T", d, j), ("I", d, j)], writes=[("T", d, j)])
                    if d == 0:
                        init = 0.0 if prev is None else Hb[0][:, c0 - 1:c0]
                        P.op("dve", lambda e, sl=sl, init=init: e.tensor_tensor_scan(Hb[0][:, sl], Ab[0][:, sl], Tb[0][:, sl], init, ALU.mult, ALU.add),
                             reads=[("A", 0, j), ("T", 0, j)] + ([("H", 0, prev)] if prev is not None else []), writes=[("H", 0, j)])
                    else:
                        rs = slice(c0 + n - 1, (c0 - 1) if c0 > 0 else None, -1)
                        if prev is None:
                            init = 0.0
                        elif prev == 0:
                            init = Hb[1][:, 0:1]
                        else:
                            init = Hb[1][:, tiles[prev][0]:tiles[prev][0] + 1]
                        P.op("dve", lambda e, rs=rs, init=init: e.tensor_tensor_scan(Hb[1][:, rs], Ab[1][:, rs], Tb[1][:, rs], init, ALU.mult, ALU.add),
                             reads=[("A", 1, j), ("T", 1, j)] + ([("H", 1, prev)] if prev is not None else []), writes=[("H", 1, j)])
                    prev = j

        def b4(b, h, par):
            gb = (b * 10 + h) % 3
            for j in range(4):
                pb = j % 2
                c0 = CT + j * 512
                for kc in range(8):
                    P.op("pe", lambda e, pb=pb, kc=kc, c0=c0, gb=gb: e.matmul(pu[pb][:], win_g[gb][:, kc, :], hT[:, kc, c0:c0 + 512],
                                                                         start=(kc == 0), stop=(kc == 7)),
                         reads=hTk + [("wing", gb)], writes=[("pu", pb)])
                P.op("act", lambda e, pb=pb, j=j: e.activation(gg[:, j * 512:(j + 1) * 512], pu[pb][:], AF.Gelu),
                     reads=[("pu", pb)], writes=[("gg", j)])
            for j in range(1, 5):
                c0, n = tiles[j]
                sl = slice(c0, c0 + n)
                P.op("dve", lambda e, sl=sl: e.tensor_tensor(Hb[0][:, sl], Hb[0][:, sl], Hb[1][:, sl], ALU.add),
                     reads=[("H", 0, j), ("H", 1, j)], writes=[("H", 0, j)])
                P.op("dve", lambda e, sl=sl, j=j: e.tensor_tensor(sbf[:, (j - 1) * 512:j * 512], Hb[0][:, sl], gg[:, (j - 1) * 512:j * 512], ALU.mult),
                     reads=[("H", 0, j), ("gg", j - 1)], writes=[("sbf", j)])
            P.dma("sp", s_s.ap()[b, h], sbf[:], reads=[("sbf", j) for j in range(1, 5)], writes=[("sbf", j) for j in range(1, 5)], ring="sts", nring=2)

        for b in range(NB):
            build_hT(b)
            for f_ in (a1, a2, a3, a4):
                f_(b, 0, 0)
            for h in range(10):
                nxt = h + 1 < 10
                np_ = (h + 1) % 2
                b1(b, h, h % 2)
                if nxt:
                    a1(b, h + 1, np_)
                b2(b, h, h % 2)
                if nxt:
                    a2(b, h + 1, np_)
                b3a(b, h, h % 2)
                if nxt:
                    a3(b, h + 1, np_)
                b3b(b, h, h % 2)
                if nxt:
                    a4(b, h + 1, np_)
                b4(b, h, h % 2)
        P.phase_end()

        P.phase_begin()
        wout = P.sb("wout", [128, 10, D], BF16)
        sT2 = [P.sb("sT%d" % i, [128, 10, S], BF16) for i in range(2)]
        bcg2b = [P.sb("bcg%d" % i, [128, D], F32) for i in range(2)]
        xts = [P.sb("xts%d" % i, [128, D], F32) for i in range(3)]
        upd = P.sb("upd", [128, 512], F32)
        py = [P.ps("py%d" % i, [128, 512], F32) for i in range(4)]
        P.dma("pool", wout[:], lru_w_out.ap().rearrange("(c p) n -> p c n", p=128), writes=["wout"], ring="ldw")
        xn = 0
        for b in range(NB):
            P.dma("sp" if b == 0 else "act", sT2[b][:], s_s.ap()[b].rearrange("h p t -> p h t"), writes=[("sT", b)], ring="ldsT")
            P.dma("act", bcg2b[b][:], modrows.ap()[b, 1, 2 * D:3 * D].partition_broadcast(128), writes=[("bcg", b)], ring="ldbc", nring=6)
        for b in range(NB):
            sT = sT2[b]
            bcg = bcg2b[b]
            for t in range(16):
                xb = xn % 3
                r0 = b * S + t * 128
                P.dma("sp", xts[xb][:], x_s.ap()[r0:r0 + 128, :], writes=[("xts", xb)], ring="ldx", nring=2)
                for hh in range(2):
                    pb = (xn % 2) * 2 + hh
                    for h in range(10):
                        P.op("pe", lambda e, pb=pb, h=h, t=t, hh=hh, sT=sT: e.matmul(py[pb][:], sT[:, h, t * 128:(t + 1) * 128], wout[:, h, hh * 512:(hh + 1) * 512],
                                                                        start=(h == 0), stop=(h == 9)),
                             reads=[("sT", b), "wout"], writes=[("py", pb)])
                    P.op("dve", lambda e, pb=pb, hh=hh, bcg=bcg: e.tensor_tensor(upd[:], py[pb][:], bcg[:, hh * 512:(hh + 1) * 512], ALU.mult),
                         reads=[("py", pb), ("bcg", b)], writes=["upd"])
                    P.op("dve", lambda e, xb=xb, hh=hh: e.tensor_tensor(xts[xb][:, hh * 512:(hh + 1) * 512], xts[xb][:, hh * 512:(hh + 1) * 512], upd[:], ALU.add),
                         reads=["upd", ("xts", xb)], writes=[("xts", xb)])
                xn += 1
                P.dma("act", x_s.ap()[r0:r0 + 128, :], xts[xb][:], reads=[("xts", xb)], ring="stx", nring=2)
        P.phase_end()

    if upto >= 3:
        NJ = 8
        CW = 2 * DE // NJ
        xs_h = dr("xs_h", [NSLOT, D], BF16)
        y_h = dr("y_h", [NSLOT, D], F32)
        idxgu_s = dr("idxgu_s", [128, NBLK * 8], I32)
        idxdn_s = dr("idxdn_s", [128, NBLK * 4], I32)
        pos_s = dr("pos_s", [128, 64], I32)
        w_s = dr("w_s", [128, 64], F32)
        NTT = NB * S // 128
        P.phase_begin()
        mc = P.sb("mc", [128, 632], F32)
        U = mc[:, 0:128]
        ones = mc[:, 128:256]
        cgu = mc[:, 256:264]
        cdn = mc[:, 320:324]
        thr = mc[:, 348:540].rearrange("p (b e) -> p b e", e=8)
        k8192 = mc[:, 540:548]
        k3584 = mc[:, 604:608]
        identf = P.sb("identf", [128, 128], F32)
        wrt = P.sb("wrt", [128, 8, NE], F32)
        bc2 = P.sb("bc2", [128, 2, D], F32)
        xts = [P.sb("xts%d" % i, [128, D], F32) for i in range(2)]
        sq = P.sb("sq", [128, D], BF16)
        tmp = P.sb("tmp", [128, D], F32)
        hm32 = P.sb("hm32", [128, D], F32)
        ssum = P.sb("ssum", [128, 1], F32)
        rstd = P.sb("rstd", [128, 1], F32)
        hmb = P.sb("hmb", [128, NTT, D], BF16)
        hmT32 = P.sb("hmT32", [128, 8, 128], F32)
        lg = P.sb("lg", [128, 8], F32)
        mx8 = P.sb("mx8", [128, 8], F32)
        negm2 = P.sb("negm2", [128, 1], F32)
        M_all = P.sb("M_all", [128, NTT, 8], F32)
        M1_all = P.sb("M1_all", [128, NTT, 8], F32)
        M2_all = P.sb("M2_all", [128, NTT, 8], F32)
        W_all = P.sb("W_all", [128, NTT, 2], F32)
        Rs = P.sb("Rs", [128, NTT, 8], F32)
        Tt = P.sb("Tt", [128, NTT, 8], F32)
        Cin = P.sb("Cin", [128, NTT, 8], F32)
        dest = P.sb("dest", [128, NTT, 8], F32)
        cnt_i = P.sb("cnt_i", [128, 8], I32)
        padf = P.sb("padf", [128, 8], F32)
        pend = P.sb("pend", [128, 8], F32)
        pstart = P.sb("pstart", [128, 8], F32)
        pos_f = P.sb("pos_f", [128, NTT, 2], F32)
        pos_i = P.sb("pos_i", [128, NTT, 2], I32)
        cmp = P.sb("cmp", [128, NBLK, 8], F32)
        be = P.sb("be", [128, NBLK], F32)
        idxgu_f = P.sb("idxgu_f", [128, NBLK, 8], F32)
        idxgu_i = P.sb("idxgu_i", [128, NBLK, 8], I32)
        idxdn_f = P.sb("idxdn_f", [128, NBLK, 4], F32)
        idxdn_i = P.sb("idxdn_i", [128, NBLK, 4], I32)
        ptf = [P.ps("ptf%d" % i, [128, 4, 128], F32) for i in range(2)]
        plg = P.ps("plg", [128, 512], F32)
        pR = P.ps("pR", [128, 512], F32)
        pT = P.ps("pT", [128, 512], F32)

        P.dma("sp", mc[:], mconst.ap(), writes=["mc"], ring="ld")
        P.dma("sp", wrt[:], w_router.ap().rearrange("(c p) e -> p c e", p=128), writes=["wrt"], ring="ld")
        P.op("pool", lambda e: e.memset(identf[:], 0.0), writes=["identf"])
        P.op("pool", lambda e: e.affine_select(identf[:], identf[:], [[-1, 128]], ALU.not_equal, 1.0, base=0, channel_multiplier=1),
             reads=["identf"], writes=["identf"])
        zt = P.sb("zt", [128, 4 * D], BF16)
        P.op("pool", lambda e: e.memset(zt[:], 0.0), writes=["zt"])
        for blk in range(NBLK):
            P.dma("pool", xs_h.ap()[blk * BLK:(blk + 1) * BLK, :].rearrange("(p t) d -> p (t d)", p=128), zt[:], reads=["zt"],
                  writes=[("xsz", blk)], ring="stz", nring=4)
        tmp2 = [tmp, P.sb("tmpB", [128, D], F32)]
        hm32_2 = [hm32, P.sb("hm32B", [128, D], F32)]
        ssum2 = [ssum, P.sb("ssumB", [128, 1], F32)]
        rstd2 = [rstd, P.sb("rstdB", [128, 1], F32)]
        hmT32_2 = [hmT32, P.sb("hmT32B", [128, 8, 128], F32)]
        lg2 = [lg, P.sb("lgB", [128, 8], F32)]
        mx82 = [mx8, P.sb("mx8B", [128, 8], F32)]
        negm22 = [negm2, P.sb("negm2B", [128, 1], F32)]
        ptf4 = ptf + [P.ps("ptf%d" % i, [128, 4, 128], F32) for i in range(2, 4)]
        plg2 = [plg, P.ps("plgB", [128, 512], F32)]
        tmp2.append(P.sb("tmpC", [128, D], F32)); hm32_2.append(P.sb("hm32C", [128, D], F32))
        ssum2.append(P.sb("ssumC", [128, 1], F32)); rstd2.append(P.sb("rstdC", [128, 1], F32))
        hmT32_2.append(P.sb("hmT32C", [128, 8, 128], F32)); lg2.append(P.sb("lgC", [128, 8], F32)); mx82.append(P.sb("mx8C", [128, 8], F32))
        xts3 = xts + [P.sb("xtsC", [128, D], F32)]
        sq3 = [sq, P.sb("sqB", [128, D], BF16), P.sb("sqC", [128, D], BF16)]

        def s1(j):
            b = j // 16
            if j % 16 == 0:
                for q in range(2):
                    P.dma("sp" if q == 0 else "act", bc2[:, q, :], modrows.ap()[b, 1, (3 + q) * D:(4 + q) * D].partition_broadcast(128),
                          writes=[("bc2", q)], ring="ldbc", nring=6)
            x3 = j % 3
            tmp_, hm_, ss_, rs_ = tmp2[x3], hm32_2[x3], ssum2[x3], rstd2[x3]
            P.dma("sp", xts3[x3][:], x_s.ap()[j * 128:(j + 1) * 128, :], writes=[("xts", x3)], ring="ldx3", nring=3)
            P.op("act", lambda e, x3=x3, ss_=ss_: e.activation(sq3[x3][:], xts3[x3][:], AF.Square, accum_out=ss_[:, 0:1]), reads=[("xts", x3)], writes=[("ssum", x3), ("sq", x3)])
            P.op("act", lambda e, ss_=ss_, rs_=rs_: e.activation(rs_[:, 0:1], ss_[:, 0:1], AF.Sqrt, bias=EPS, scale=1.0 / D), reads=[("ssum", x3)], writes=[("rstd", x3)])
            P.op("dve", lambda e, rs_=rs_: e.reciprocal(rs_[:, 0:1], rs_[:, 0:1]), reads=[("rstd", x3)], writes=[("rstd", x3)])
            P.op("dve", lambda e, x3=x3, rs_=rs_, tmp_=tmp_: e.scalar_tensor_tensor(tmp_[:], xts3[x3][:], rs_[:, 0:1], bc2[:, 1, :], ALU.mult, ALU.mult),
                 reads=[("xts", x3), ("rstd", x3), ("bc2", 1)], writes=[("tmp", x3)])
            P.op("dve", lambda e, tmp_=tmp_, hm_=hm_: e.tensor_tensor(hm_[:], tmp_[:], bc2[:, 0, :], ALU.add), reads=[("tmp", x3), ("bc2", 0)], writes=[("hm32", x3)])

        def s2(j):
            x3 = j % 3
            xb = j % 2
            hm_, hT_, pl_ = hm32_2[x3], hmT32_2[x3], plg2[xb]
            P.op("act", lambda e, j=j, hm_=hm_: e.activation(hmb[:, j, :], hm_[:], AF.Copy), reads=[("hm32", x3)], writes=[("hmb", j)])
            for c in range(8):
                P.op("pe", lambda e, c=c, hm_=hm_, xb=xb: e.transpose(ptf4[xb * 2 + c // 4][:, c % 4, :], hm_[:, c * 128:(c + 1) * 128], identf[:]),
                     reads=[("hm32", x3), "identf"], writes=[("ptf", xb * 2 + c // 4)])
            for h2 in range(2):
                P.op("dve" if h2 == 0 else "act",
                     (lambda e, h2=h2, hT_=hT_, xb=xb: e.tensor_copy(hT_[:, h2 * 4:(h2 + 1) * 4, :], ptf4[xb * 2 + h2][:])) if h2 == 0 else
                     (lambda e, h2=h2, hT_=hT_, xb=xb: e.activation(hT_[:, h2 * 4:(h2 + 1) * 4, :], ptf4[xb * 2 + h2][:], AF.Copy)),
                     reads=[("ptf", xb * 2 + h2)], writes=[("hmT32", x3, h2)])
            for c in range(8):
                P.op("pe", lambda e, c=c, hT_=hT_, pl_=pl_: e.matmul(pl_[:, 0:8], hT_[:, c, :], wrt[:, c, :], start=(c == 0), stop=(c == 7)),
                     reads=[("hmT32", x3, c // 4), "wrt"], writes=[("plg", xb)])

        def s3(j):
            x3 = j % 3
            xb = j % 2
            lg_, mx_, pl_ = lg2[x3], mx82[x3], plg2[xb]
            P.op("act", lambda e, lg_=lg_, pl_=pl_: e.activation(lg_[:], pl_[:, 0:8], AF.Copy), reads=[("plg", xb)], writes=[("lg", x3)])
            P.op("dve", lambda e, lg_=lg_, mx_=mx_: e.max(mx_[:], lg_[:]), reads=[("lg", x3)], writes=[("mx8", x3)])
            P.op("dve", lambda e, j=j, lg_=lg_, mx_=mx_: e.tensor_scalar(M1_all[:, j, :], lg_[:], mx_[:, 0:1], None, ALU.is_equal), reads=[("lg", x3), ("mx8", x3)], writes=[("M1", j)])
            P.op("dve", lambda e, j=j, lg_=lg_, mx_=mx_: e.tensor_scalar(M2_all[:, j, :], lg_[:], mx_[:, 1:2], None, ALU.is_equal), reads=[("lg", x3), ("mx8", x3)], writes=[("M2", j)])
            P.op("dve", lambda e, j=j, mx_=mx_: e.tensor_tensor(W_all[:, j, 0:1], mx_[:, 0:1], mx_[:, 1:2], ALU.subtract), reads=[("mx8", x3)], writes=[("W", j)])

        for it in range(NTT + 2):
            if it < NTT:
                s1(it)
            if 0 <= it - 1 < NTT:
                s2(it - 1)
            if 0 <= it - 2 < NTT:
                s3(it - 2)
        allW = [("W", j) for j in range(NTT)]
        P.op("act", lambda e: e.activation(W_all[:, :, 0], W_all[:, :, 0], AF.Sigmoid), reads=allW, writes=allW)
        P.op("dve", lambda e: e.tensor_scalar(W_all[:, :, 1], W_all[:, :, 0], -1.0, 1.0, ALU.mult, ALU.add), reads=allW, writes=allW)
        allM1 = [("M1", j) for j in range(NTT)]
        allM2 = [("M2", j) for j in range(NTT)]
        fl = lambda t: t[:].rearrange("p j e -> p (j e)")
        P.op("dve", lambda e: e.tensor_tensor(fl(M_all), fl(M1_all), fl(M2_all), ALU.add), reads=allM1 + allM2, writes=["M"])
        P.op("pe", lambda e: e.matmul(pR[:, 0:256], U, fl(M_all), start=True, stop=True), reads=["M", "mc"], writes=["pR"])
        P.op("pe", lambda e: e.matmul(pT[:, 0:256], ones, fl(M_all), start=True, stop=True), reads=["M", "mc"], writes=["pT"])
        P.op("act", lambda e: e.activation(fl(Rs), pR[:, 0:256], AF.Copy), reads=["pR"], writes=["Rs"])
        P.op("dve", lambda e: e.tensor_copy(fl(Tt), pT[:, 0:256]), reads=["pT"], writes=["Tt"])
        for ee in range(8):
            P.op("dve", lambda e, ee=ee: e.tensor_tensor_scan(Cin[:, :, ee], ones[:, 0:NTT], Tt[:, :, ee], 0.0, ALU.mult, ALU.add),
                 reads=["Tt", "mc"], writes=["Cin"])
        P.op("dve", lambda e: e.tensor_copy(cnt_i[:], Cin[:, NTT - 1, :]), reads=["Cin"], writes=["cnt_i"])
        P.op("dve", lambda e: e.tensor_scalar(cnt_i[:], cnt_i[:], BLK - 1, None, ALU.add), reads=["cnt_i"], writes=["cnt_i"])
        P.op("dve", lambda e: e.tensor_scalar(cnt_i[:], cnt_i[:], 9, 9, ALU.arith_shift_right, ALU.logical_shift_left), reads=["cnt_i"], writes=["cnt_i"])
        P.op("dve", lambda e: e.tensor_copy(padf[:], cnt_i[:]), reads=["cnt_i"], writes=["padf"])
        P.op("dve", lambda e: e.tensor_tensor_scan(pend[:], ones[:, 0:8], padf[:], 0.0, ALU.mult, ALU.add), reads=["padf", "mc"], writes=["pend"])
        P.op("dve", lambda e: e.tensor_tensor(pstart[:], pend[:], padf[:], ALU.subtract), reads=["pend", "padf"], writes=["pstart"])
        P.op("dve", lambda e: e.tensor_tensor(fl(dest), fl(Cin), fl(Tt), ALU.subtract), reads=["Cin", "Tt"], writes=["dest"])
        P.op("dve", lambda e: e.tensor_tensor(fl(dest), fl(dest), fl(Rs), ALU.add), reads=["dest", "Rs"], writes=["dest"])
        P.op("dve", lambda e: e.tensor_tensor(dest[:], dest[:], pstart[:].unsqueeze(1).to_broadcast([128, NTT, 8]), ALU.add), reads=["dest", "pstart"], writes=["dest"])
        P.op("dve", lambda e: e.tensor_tensor(fl(M1_all), fl(M1_all), fl(dest), ALU.mult), reads=allM1 + ["dest", "M"], writes=allM1)
        P.op("dve", lambda e: e.tensor_tensor(fl(M2_all), fl(M2_all), fl(dest), ALU.mult), reads=allM2 + ["dest", "M"], writes=allM2)
        P.op("dve", lambda e: e.tensor_reduce(pos_f[:, :, 0], M1_all[:], AX.X, ALU.add), reads=allM1, writes=["pos_f0"])
        P.op("dve", lambda e: e.tensor_reduce(pos_f[:, :, 1], M2_all[:], AX.X, ALU.add), reads=allM2, writes=["pos_f1"])
        P.op("dve", lambda e: e.tensor_copy(pos_i[:], pos_f[:]), reads=["pos_f0", "pos_f1"], writes=["pos_i"])
        P.op("dve", lambda e: e.tensor_tensor(cmp[:], thr, pend[:].unsqueeze(1).to_broadcast([128, NBLK, 8]), ALU.is_ge), reads=["mc", "pend"], writes=["cmp"])
        P.op("dve", lambda e: e.tensor_reduce(be[:], cmp[:], AX.X, ALU.add), reads=["cmp"], writes=["be"])
        P.op("dve", lambda e: e.tensor_scalar(be[:], be[:], float(NE - 1), None, ALU.min), reads=["be"], writes=["be"])
        for blk in range(NBLK):
            P.op("dve", lambda e, blk=blk: e.scalar_tensor_tensor(idxgu_f[:, blk, :], k8192, be[:, blk:blk + 1], cgu, ALU.mult, ALU.add),
                 reads=["be", "mc"], writes=["idxgu_f"])
            P.op("dve", lambda e, blk=blk: e.scalar_tensor_tensor(idxdn_f[:, blk, :], k3584, be[:, blk:blk + 1], cdn, ALU.mult, ALU.add),
                 reads=["be", "mc"], writes=["idxdn_f"])
        P.op("dve", lambda e: e.tensor_copy(idxgu_i[:], idxgu_f[:]), reads=["idxgu_f"], writes=["idxgu_i"])
        P.op("dve", lambda e: e.tensor_copy(idxdn_i[:], idxdn_f[:]), reads=["idxdn_f"], writes=["idxdn_i"])
        P.dma("sp", idxgu_s.ap(), idxgu_i[:].rearrange("p b c -> p (b c)"), reads=["idxgu_i"], ring="st")
        P.dma("sp", idxdn_s.ap(), idxdn_i[:].rearrange("p b c -> p (b c)"), reads=["idxdn_i"], ring="st")
        P.dma("sp", pos_s.ap(), pos_i[:].rearrange("p j k -> p (j k)"), reads=["pos_i"], ring="st")
        P.dma("sp", w_s.ap(), W_all[:].rearrange("p j k -> p (j k)"), reads=[("W", j) for j in range(NTT)], ring="st")
        for j in range(NTT):
            for k in range(2):
                P.scatter(xs_h.ap()[:, :], hmb[:, j, :], pos_i[:, j, k:k + 1], reads=[("hmb", j), "pos_i"] + [("xsz", blk) for blk in range(NBLK)], writes=[("xs", j, k)])
        if debug:
            dlg = dr("dbg_be", [128, NBLK], F32, kind="ExternalOutput")
            P.dma("sp", dlg.ap(), be[:], reads=["be"], ring="st")
            dps = dr("dbg_pos", [128, 64], F32, kind="ExternalOutput")
            P.dma("sp", dps.ap(), pos_f[:].rearrange("p j k -> p (j k)"), reads=["pos_f0", "pos_f1"], ring="st")
            dpe = dr("dbg_pend", [128, 8], F32, kind="ExternalOutput")
            P.dma("sp", dpe.ap(), pend[:], reads=["pend"], ring="st")
        P.phase_end()

        P.phase_begin()
        idxgu = P.sb("idxgu", [128, NBLK, 8], I32)
        idxdn = P.sb("idxdn", [128, NBLK, 4], I32)
        identf = P.sb("identf", [128, 128], F32)
        ident = P.sb("ident", [128, 128], BF16)
        xrs = [P.sb("xr%d" % i, [128, 4, D], BF16) for i in range(2)]
        xsT = P.sb("xsT", [128, 8, 512], BF16)
        wg = [P.sb("wg%d" % i, [128, 2, 4, 2 * CW], BF16) for i in range(3)]
        wd = P.sb("wd", [128, 4, 7, D], BF16)
        act = P.sb("act", [128, 28, 512], BF16)
        sg = [P.sb("sg%d" % i, [128, 512], BF16) for i in range(2)]
        yt = [P.sb("yt%d" % i, [128, D], F32) for i in range(2)]
        pp = [P.ps("pp%d" % i, [128, 512], F32) for i in range(4)]
        py = [P.ps("py%d" % i, [128, 512], F32) for i in range(2)]
        ptrs = [P.ps("ptr%d" % i, [128, 8, 128], BF16) for i in range(2)]
        P.dma("sp", idxgu[:].rearrange("p b c -> p (b c)"), idxgu_s.ap(), writes=["idxgu"], ring="ld")
        P.dma("sp", idxdn[:].rearrange("p b c -> p (b c)"), idxdn_s.ap(), writes=["idxdn"], ring="ld")
        P.op("pool", lambda e: e.memset(identf[:], 0.0), writes=["identf"])
        P.op("pool", lambda e: e.affine_select(identf[:], identf[:], [[-1, 128]], ALU.not_equal, 1.0, base=0, channel_multiplier=1),
             reads=["identf"], writes=["identf"])
        P.op("dve", lambda e: e.tensor_copy(ident[:], identf[:]), reads=["identf"], writes=["ident"])
        wgn = 0
        ytn = 0

        def gu_stage_load(blk, s, wb):
            for h2 in range(2):
                col = s * 2 + h2
                P.gather(wg[wb][:, h2, :, :].rearrange("p k c -> p (k c)"), moe_gu.ap()[:, :], idxgu[:, blk, col:col + 1],
                         reads=["idxgu"], writes=[("wg", wb, h2)], ring="ig", nring=4)

        def xr_load(blk):
            P.dma("sp", xrs[blk % 2][:], xs_h.ap()[blk * BLK:(blk + 1) * BLK, :].rearrange("(t p) d -> p t d", p=128),
                  writes=[("xr", blk % 2)], ring="ldx", nring=2)

        xr_load(0)
        NST = NBLK * 4
        for n0 in range(2):
            gu_stage_load(n0 // 4, n0 % 4, n0 % 3)
        def do_transposes(blk):
            xr = xrs[blk % 2]
            for t in range(4):
                ptr = ptrs[t % 2]
                for c in range(8):
                    P.op("pe", lambda e, t=t, c=c, xr=xr, ptr=ptr: e.transpose(ptr[:, c, :], xr[:, t, c * 128:(c + 1) * 128], ident[:]),
                         reads=[("xr", blk % 2), "ident"], writes=[("ptr", t % 2)])
                P.op("act", lambda e, t=t, ptr=ptr: e.activation(xsT[:, :, t * 128:(t + 1) * 128], ptr[:], AF.Copy), reads=[("ptr", t % 2)], writes=[("xsT", t)])

        do_transposes(0)
        for blk in range(NBLK):
            if blk + 1 < NBLK:
                xr_load(blk + 1)
            xk = [("xsT", t) for t in range(4)]
            for s in range(4):
                n = blk * 4 + s
                if n + 2 < NST:
                    gu_stage_load((n + 2) // 4, (n + 2) % 4, (n + 2) % 3)
                if s == 0:
                    for q in range(4):
                        P.gather(wd[:, q, :, :].rearrange("p f n -> p (f n)"), moe_dn.ap()[:, :], idxdn[:, blk, q:q + 1],
                                 reads=["idxdn"], writes=[("wd", q)], ring="igd", nring=4)
                wb = n % 3
                for fi in range(7):
                    ft = s * 7 + fi
                    pb = (ft % 2) * 2
                    for half in range(2):
                        for kc in range(8):
                            P.op("pe", lambda e, wb=wb, half=half, kc=kc, fi=fi, pb=pb: e.matmul(
                                pp[pb + half][:], wg[wb][:, kc // 4, kc % 4, half * CW + fi * 128:half * CW + (fi + 1) * 128], xsT[:, kc, :],
                                start=(kc == 0), stop=(kc == 7)),
                                reads=xk + [("wg", wb, kc // 4)], writes=[("pp", pb + half)])
                    sgi = ft % 2
                    P.op("act", lambda e, pb=pb, sgi=sgi: e.activation(sg[sgi][:], pp[pb][:], AF.Silu), reads=[("pp", pb)], writes=[("sg", sgi)])
                    P.op("dve", lambda e, pb=pb, sgi=sgi, ft=ft: e.tensor_tensor(act[:, ft, :], sg[sgi][:], pp[pb + 1][:], ALU.mult),
                         reads=[("sg", sgi), ("pp", pb + 1)], writes=[("act", ft)])
            if blk + 1 < NBLK:
                do_transposes(blk + 1)
            actk = [("act", ft) for ft in range(28)]
            for t in range(4):
                yb = ytn % 2
                ytn += 1
                for hh in range(2):
                    for ft in range(28):
                        P.op("pe", lambda e, t=t, hh=hh, ft=ft: e.matmul(py[hh][:], act[:, ft, t * 128:(t + 1) * 128], wd[:, ft // 7, ft % 7, hh * 512:(hh + 1) * 512],
                                                                        start=(ft == 0), stop=(ft == 27)),
                             reads=actk + [("wd", ft // 7)], writes=[("py", hh)])
                    if hh == 0:
                        P.op("act", lambda e, yb=yb: e.activation(yt[yb][:, 0:512], py[0][:], AF.Copy), reads=[("py", 0)], writes=[("yt", yb, 0)])
                    else:
                        P.op("dve", lambda e, yb=yb: e.tensor_copy(yt[yb][:, 512:1024], py[1][:]), reads=[("py", 1)], writes=[("yt", yb, 1)])
                r0 = blk * BLK + t * 128
                P.dma("sp", y_h.ap()[r0:r0 + 128, :], yt[yb][:], reads=[("yt", yb, 0), ("yt", yb, 1)], ring="sty", nring=2)
        P.phase_end()

        P.phase_begin()
        pos_i = P.sb("pos_i", [128, NTT, 2], I32)
        W_all = P.sb("W_all", [128, NTT, 2], F32)
        bcg2 = P.sb("bcg2", [128, D], F32)
        fg = P.sb("fg", [128, D], F32)
        xts = [P.sb("xts%d" % i, [128, D], F32) for i in range(4)]
        y1 = [P.sb("y1_%d" % i, [128, D], F32) for i in range(4)]
        y2 = [P.sb("y2_%d" % i, [128, D], F32) for i in range(4)]
        acc = P.sb("acc", [128, D], F32)
        sq = P.sb("sq", [128, D], BF16)
        ot = [P.sb("ot%d" % i, [128, D], F32) for i in range(4)]
        ssum = P.sb("ssum", [128, 1], F32)
        rstd = P.sb("rstd", [128, 1], F32)
        P.dma("sp", pos_i[:].rearrange("p j k -> p (j k)"), pos_s.ap(), writes=["pos_i"], ring="ld")
        P.dma("sp", W_all[:].rearrange("p j k -> p (j k)"), w_s.ap(), writes=["W_all"], ring="ld")
        P.dma("act", fg[:], final_g.ap().partition_broadcast(128), writes=["fg"], ring="ld")
        sq4 = [sq] + [P.sb("sq%d" % i, [128, D], BF16) for i in range(1, 4)]
        acc2 = [acc] + [P.sb("acc%d" % i, [128, D], F32) for i in range(1, 4)]
        ssum2 = [ssum] + [P.sb("ssum%d" % i, [128, 1], F32) for i in range(1, 4)]
        rstd2 = [rstd] + [P.sb("rstd%d" % i, [128, 1], F32) for i in range(1, 4)]
        def c_front(j):
            b = j // 16
            i2 = j % 4
            ac_ = acc2[i2]
            if j % 16 == 0:
                P.dma("act", bcg2[:], modrows.ap()[b, 1, 5 * D:6 * D].partition_broadcast(128), writes=["bcg2"], ring="ldbc", nring=6)
            P.dma("sp", xts[i2][:], x_s.ap()[j * 128:(j + 1) * 128, :], writes=[("xts", i2)], ring="ldx4", nring=4)
            P.gather(y1[i2][:, :], y_h.ap()[:, :], pos_i[:, j, 0:1], reads=["pos_i"], writes=[("y1", i2)], ring="ig", nring=8)
            P.gather(y2[i2][:, :], y_h.ap()[:, :], pos_i[:, j, 1:2], reads=["pos_i"], writes=[("y2", i2)], ring="ig", nring=8)
            P.op("act", lambda e, i2=i2, j=j, ac_=ac_: e.activation(ac_[:], y1[i2][:], AF.Copy, scale=W_all[:, j, 0:1]), reads=[("y1", i2), "W_all"], writes=[("acc", i2)])
            P.op("dve", lambda e, i2=i2, j=j, ac_=ac_: e.scalar_tensor_tensor(ac_[:], y2[i2][:], W_all[:, j, 1:2], ac_[:], ALU.mult, ALU.add),
                 reads=[("y2", i2), "W_all", ("acc", i2)], writes=[("acc", i2)])
            P.op("dve", lambda e, ac_=ac_: e.tensor_tensor(ac_[:], ac_[:], bcg2[:], ALU.mult), reads=[("acc", i2), "bcg2"], writes=[("acc", i2)])
            P.op("dve", lambda e, i2=i2, ac_=ac_: e.tensor_tensor(xts[i2][:], xts[i2][:], ac_[:], ALU.add), reads=[("acc", i2), ("xts", i2)], writes=[("xts", i2)])

        def c_back(j):
            i2 = j % 4
            ss_, rs_ = ssum2[i2], rstd2[i2]
            P.op("act", lambda e, i2=i2, ss_=ss_: e.activation(sq4[i2][:], xts[i2][:], AF.Square, accum_out=ss_[:, 0:1]), reads=[("xts", i2)], writes=[("ssum", i2), ("sq", i2)])
            P.op("act", lambda e, ss_=ss_, rs_=rs_: e.activation(rs_[:, 0:1], ss_[:, 0:1], AF.Sqrt, bias=EPS, scale=1.0 / D), reads=[("ssum", i2)], writes=[("rstd", i2)])
            P.op("dve", lambda e, rs_=rs_: e.reciprocal(rs_[:, 0:1], rs_[:, 0:1]), reads=[("rstd", i2)], writes=[("rstd", i2)])
            if upto >= 4:
                P.op("dve", lambda e, i2=i2, rs_=rs_: e.scalar_tensor_tensor(ot[i2][:], xts[i2][:], rs_[:, 0:1], fg[:], ALU.mult, ALU.mult),
                     reads=[("xts", i2), ("rstd", i2), "fg"], writes=[("ot", i2)])
                P.dma("act", out_h.ap()[j * 128:(j + 1) * 128, :], ot[i2][:], reads=[("ot", i2)], ring="sto4", nring=4)
            else:
                P.dma("act", x_s.ap()[j * 128:(j + 1) * 128, :], xts[i2][:], reads=[("xts", i2)], ring="sto4", nring=4)

        c_front(0)
        for j in range(NTT):
            if j + 1 < NTT:
                c_front(j + 1)
            c_back(j)
        P.phase_end()

    if debug:
        P.phase_begin()
        dx = dr("dbg_x", [NB * S, D], F32, kind="ExternalOutput")
        dz = dr("dbg_z", [NB * CT, D], F32, kind="ExternalOutput")
        dm = dr("dbg_mod", [3, 2, 6 * D], F32, kind="ExternalOutput")
        P.dma("sp", dx.ap(), x_s.ap(), ring="st")
        P.dma("sp", dz.ap(), z_s.ap(), ring="st")
        P.dma("sp", dm.ap(), modrows.ap(), ring="st")
        P.phase_end()
    elif upto < 4:
        P.phase_begin()
        P.dma("sp", out_h.ap(), x_s.ap(), ring="st")
        P.phase_end()
    return P.finish()


def _moe_consts():
    mc = np.zeros((128, 632), np.float32)
    p = np.arange(128)
    mc[:, 0:128] = (p[:, None] < p[None, :]).astype(np.float32)
    mc[:, 128:256] = 1.0
    for s_ in range(4):
        for h in range(2):
            mc[:, 256 + s_ * 2 + h] = (p * 4 + s_) * 2 + h
    for q in range(4):
        mc[:, 320 + q] = p * 4 + q
    for blk in range(NBLK):
        mc[:, 348 + blk * 8:348 + (blk + 1) * 8] = BLK * blk
    mc[:, 540:604] = 1024.0
    mc[:, 604:632] = 512.0
    return mc


def _ffn_gu_layout(w):
    w5 = w.reshape(8, 128, 2, 11, 256)
    w5 = w5.transpose(1, 3, 2, 0, 4)
    return np.ascontiguousarray(w5).reshape(128, 11, 2 * 8 * 256)


def _gu_layout(w):
    w6 = w.reshape(NE, 2, 4, 128, 2, 4, 896)
    w6 = w6.transpose(0, 3, 5, 1, 2, 4, 6)
    return np.ascontiguousarray(w6).reshape(NE * 128 * 8, 4 * 2 * 896)


def _dn_layout(w):
    w5 = w.reshape(NE, 4, 7, 128, D)
    w5 = w5.transpose(0, 3, 1, 2, 4)
    return np.ascontiguousarray(w5).reshape(NE * 128 * 4, 7 * D)


def make_in_maps(inputs):
    f = lambda k: np.ascontiguousarray(np.asarray(inputs[k], dtype=np.float32))
    x = f("x"); c = f("c"); ctx = f("ctx"); c_ctx = f("c_ctx")
    atx, atz = _pool_mats()
    conv_w = f("lru_conv_w")[0]; conv_b = f("lru_conv_b")[0]
    b_r = f("lru_b_r")[0]; b_i = f("lru_b_i")[0]; lam = f("lru_lambda")[0]
    pp = np.zeros((1280, 12), np.float32)
    pp[:, 0:4] = conv_w.T
    pp[:, 4] = conv_b
    pp[:, 5:7] = b_r.T
    pp[:, 7:9] = b_i.T
    pp[:, 9:11] = lam.T
    lru_pp = np.ascontiguousarray(pp.reshape(10, 128, 12).transpose(1, 0, 2))
    shared = {
        "ada_w": f("ada_w"), "ada_b": f("ada_b"), "norm1_g": f("norm1_g"), "norm2_g": f("norm2_g"),
        "pool_w": f("pool_w")[0], "pool_scale": f("pool_scale")[0],
        "ffn_w_gu": _ffn_gu_layout(f("ffn_w_gu")[0]), "ffn_w_down": f("ffn_w_down")[0],
        "atx": atx, "atz": atz,
        "lru_w_in": f("lru_w_in")[0], "lru_pp": lru_pp,
        "lru_w_r": f("lru_w_r")[0], "lru_w_i": f("lru_w_i")[0], "lru_w_out": f("lru_w_out")[0],
        "moe_w_router": f("moe_w_router")[0],
        "moe_w_gu": _gu_layout(f("moe_w_gu")[0]), "moe_w_down": _dn_layout(f("moe_w_down")[0]),
        "final_g": f("final_g"), "mconst": _moe_consts(),
    }
    maps = []
    for core in range(8):
        b0 = core * NB
        c3 = np.stack([c[b0], c[b0 + 1], c_ctx], 0)
        cT = np.ascontiguousarray(c3.reshape(3, 8, 128).transpose(2, 1, 0))
        m = dict(shared)
        m["x"] = x[b0:b0 + NB].reshape(NB * S, D)
        m["ctx"] = ctx[b0:b0 + NB].reshape(NB * CT, D)
        m["cT"] = cT
        maps.append(m)
    return maps


_USED = None


def kernel(**inputs):
    nc = build()
    maps = make_in_maps(inputs)
    used = set(nc_input_names(nc))
    maps = [{k: v for k, v in m.items() if k in used} for m in maps]
    res = run_bass_kernel_spmd(nc, maps, core_ids=list(range(8)))
    out = np.stack([r["out"].reshape(NB, S, D) for r in res.results], 0).reshape(16, S, D)
    return out.astype(np.float32)


def nc_input_names(nc):
    return ["x", "ctx", "cT", "ada_w", "ada_b", "norm1_g", "norm2_g", "pool_w", "pool_scale", "ffn_w_gu",
            "ffn_w_down", "atx", "atz", "lru_w_in", "lru_pp", "lru_w_r", "lru_w_i", "lru_w_out",
            "moe_w_router", "moe_w_gu", "moe_w_down", "final_g", "mconst"]
```

```python
import numpy as np
from contextlib import ExitStack
import concourse.bass as bass
import concourse.mybir as mybir
from concourse.bass_utils import run_bass_kernel_spmd

F32 = mybir.dt.float32
BF16 = mybir.dt.bfloat16
I32 = mybir.dt.int32
ALU = mybir.AluOpType
AF = mybir.ActivationFunctionType
AX = mybir.AxisListType

ENGS = ("pe", "act", "dve", "pool", "sp")
NB = 2
S = 2048
D = 1024
CT = 256
DFF = 2816
DR = 1280
DE = 3584
NE = 8
BLK = 512
NBLK = (NB * S * 2) // BLK + NE
NSLOT = NBLK * BLK
EPS = 1e-6


class Prog:
    def __init__(self):
        self.nc = bass.Bass("TRN2", target_bir_lowering=False)
        self.es = ExitStack()
        self.pes = None
        self.q = {e: [] for e in ENGS}
        self.cnt = {e: 0 for e in ENGS}
        self.esem = {e: self.es.enter_context(self.nc.semaphore("es_" + e)) for e in ENGS}
        self.seen = {e: {} for e in ENGS}
        self.res = {}
        self.rings = {}
        self.dcnt = {}
        self.semobj = {}
        for e in ENGS:
            self.semobj[id(self.esem[e])] = self.esem[e]
        self.n_inst = 0
        self.uid = 0

    def sb(self, name, shape, dt):
        self.uid += 1
        return self.pes.enter_context(self.nc.sbuf_tensor("s%d_%s" % (self.uid, name), list(shape), dt))

    def ps(self, name, shape, dt=F32):
        self.uid += 1
        return self.pes.enter_context(self.nc.psum_tensor("p%d_%s" % (self.uid, name), list(shape), dt))

    def dram(self, name, shape, dt, kind="Internal"):
        return self.nc.dram_tensor(name, list(shape), dt, kind=kind)

    def _st(self, k):
        s = self.res.get(k)
        if s is None:
            s = {"w": None, "r": {}}
            self.res[k] = s
        return s

    def _waits(self, eng, reads, writes):
        need = {}

        def add(tok):
            if tok is None:
                return
            sid, val = tok
            if need.get(sid, 0) < val:
                need[sid] = val

        for r in reads:
            add(self._st(r)["w"])
        for w in writes:
            s = self._st(w)
            add(s["w"])
            for sid, val in s["r"].items():
                add((sid, val))
        own = id(self.esem[eng])
        for sid, val in need.items():
            if eng == "pe" and sid == own:
                continue
            if self.seen[eng].get(sid, 0) >= val:
                continue
            self.seen[eng][sid] = val
            self.q[eng].append(("wait", self.semobj[sid], val))

    def _commit(self, tok, reads, writes):
        sid, val = tok
        for r in reads:
            s = self._st(r)
            if s["r"].get(sid, 0) < val:
                s["r"][sid] = val
        for w in writes:
            s = self._st(w)
            s["w"] = tok
            s["r"] = {}

    def op(self, eng, fn, reads=(), writes=()):
        self._waits(eng, reads, writes)
        self.cnt[eng] += 1
        self.q[eng].append(("inst", fn, self.esem[eng], 1))
        self._commit((id(self.esem[eng]), self.cnt[eng]), reads, writes)
        self.n_inst += 1

    def _ring(self, ring, nring):
        rg = self.rings.get(ring)
        if rg is None:
            rg = {"sems": [self.es.enter_context(self.nc.semaphore("dq_%s_%d" % (ring, i)))
                           for i in range(nring)], "i": 0}
            for s in rg["sems"]:
                self.semobj[id(s)] = s
                self.dcnt[id(s)] = 0
            self.rings[ring] = rg
        sem = rg["sems"][rg["i"] % len(rg["sems"])]
        rg["i"] += 1
        return sem

    def dmafn(self, eng, fn, reads=(), writes=(), ring="d", nring=4):
        sem = self._ring(ring, nring)
        sid = id(sem)
        self._waits(eng, reads, writes)
        if self.dcnt[sid] > 0 and self.seen[eng].get(sid, 0) < self.dcnt[sid]:
            self.seen[eng][sid] = self.dcnt[sid]
            self.q[eng].append(("wait", sem, self.dcnt[sid]))
        self.dcnt[sid] += 16
        self.q[eng].append(("inst", fn, sem, 16))
        self._commit((sid, self.dcnt[sid]), reads, writes)
        self.n_inst += 1

    def dma(self, eng, out, in_, reads=(), writes=(), ring="d", nring=4, **kw):
        self.dmafn(eng, lambda e, o=out, i=in_, k=kw: e.dma_start(out=o, in_=i, **k),
                   reads, writes, ring, nring)

    def gather(self, out, src, idx, reads=(), writes=(), ring="ig", nring=4):
        self.dmafn("pool", lambda e, o=out, s=src, i=idx: e.indirect_dma_start(
            out=o, out_offset=None, in_=s, in_offset=bass.IndirectOffsetOnAxis(ap=i, axis=0)),
            reads, writes, ring, nring)

    def scatter(self, dst, in_, idx, reads=(), writes=(), ring="is", nring=4):
        self.dmafn("pool", lambda e, o=dst, s=in_, i=idx: e.indirect_dma_start(
            out=o, out_offset=bass.IndirectOffsetOnAxis(ap=i, axis=0), in_=s, in_offset=None),
            reads, writes, ring, nring)

    def phase_begin(self):
        self.pes = ExitStack()

    def _emit(self):
        q = self.q

        def replay(lst, e):
            for it in lst:
                if it[0] == "wait":
                    e.wait_ge(it[1], it[2])
                else:
                    it[1](e).then_inc(it[2], it[3])

        with self.nc.Block() as block:
            @block.tensor
            def _(e):
                replay(q["pe"], e)

            @block.scalar
            def _(e):
                replay(q["act"], e)

            @block.vector
            def _(e):
                replay(q["dve"], e)

            @block.gpsimd
            def _(e):
                replay(q["pool"], e)

            @block.sync
            def _(e):
                replay(q["sp"], e)
        self.q = {e: [] for e in ENGS}

    def phase_end(self):
        for e in ENGS:
            for e2 in ENGS:
                if e2 != e and self.cnt[e2] > self.seen[e].get(id(self.esem[e2]), 0):
                    if e == "pe" and e2 == "pe":
                        continue
                    self.seen[e][id(self.esem[e2])] = self.cnt[e2]
                    self.q[e].append(("wait", self.esem[e2], self.cnt[e2]))
            if e != "pe" and self.cnt[e] > self.seen[e].get(id(self.esem[e]), 0):
                self.seen[e][id(self.esem[e])] = self.cnt[e]
                self.q[e].append(("wait", self.esem[e], self.cnt[e]))
            for sid, c in self.dcnt.items():
                if c > self.seen[e].get(sid, 0):
                    self.seen[e][sid] = c
                    self.q[e].append(("wait", self.semobj[sid], c))
        self.res = {}
        self._emit()
        self.pes.close()
        self.pes = None

    def finish(self):
        self.es.close()
        return self.nc


def _pool_mats():
    def amat(n, win):
        A = np.zeros((n, n), np.float64)
        for t in range(n):
            lo = min(max(t - win // 2, 0), n)
            hi = min(max(t - win // 2 + win, 0), n)
            A[t, lo:hi] = 1.0 / (hi - lo)
        return A - np.eye(n)
    atx = np.zeros((128, 4, 128), np.float32)
    atz = np.zeros((128, 4, 2, 256), np.float32)
    for g, win in enumerate((2, 4, 8, 16)):
        a64 = amat(64, win)
        blk = np.zeros((128, 128))
        blk[:64, :64] = a64
        blk[64:, 64:] = a64
        atx[:, g, :] = blk.T
        a256 = amat(256, win).T
        atz[:, g, 0, :] = a256[:128]
        atz[:, g, 1, :] = a256[128:]
    return atx, atz


def build(upto=9, debug=False):
    P = Prog()
    nc = P.nc
    dr = P.dram
    x_in = dr("x", [NB * S, D], F32, kind="ExternalInput")
    z_in = dr("ctx", [NB * CT, D], F32, kind="ExternalInput")
    cT_in = dr("cT", [128, 8, 3], F32, kind="ExternalInput")
    ada_w = dr("ada_w", [2, D, 6 * D], F32, kind="ExternalInput")
    ada_b = dr("ada_b", [2, 6 * D], F32, kind="ExternalInput")
    n1g = dr("norm1_g", [2, D], F32, kind="ExternalInput")
    n2g = dr("norm2_g", [2, D], F32, kind="ExternalInput")
    pool_w = dr("pool_w", [4, 256, 256], F32, kind="ExternalInput")
    pool_scale = dr("pool_scale", [D], F32, kind="ExternalInput")
    w_gu = dr("ffn_w_gu", [128, 11, 2 * 8 * 256], F32, kind="ExternalInput")
    w_dn = dr("ffn_w_down", [DFF, D], F32, kind="ExternalInput")
    atx_in = dr("atx", [128, 4, 128], F32, kind="ExternalInput")
    atz_in = dr("atz", [128, 4, 2, 256], F32, kind="ExternalInput")
    if upto >= 2:
        lru_w_in = dr("lru_w_in", [D, 2 * DR], F32, kind="ExternalInput")
        lru_pp = dr("lru_pp", [128, 10, 12], F32, kind="ExternalInput")
        lru_w_r = dr("lru_w_r", [2, 10, 128, 128], F32, kind="ExternalInput")
        lru_w_i = dr("lru_w_i", [2, 10, 128, 128], F32, kind="ExternalInput")
        lru_w_out = dr("lru_w_out", [DR, D], F32, kind="ExternalInput")
    if upto >= 3:
        mconst = dr("mconst", [128, 632], F32, kind="ExternalInput")
        w_router = dr("moe_w_router", [D, NE], F32, kind="ExternalInput")
        moe_gu = dr("moe_w_gu", [NE * 128 * 8, 4 * 2 * 896], F32, kind="ExternalInput")
        moe_dn = dr("moe_w_down", [NE * 128 * 4, 7 * D], F32, kind="ExternalInput")
    final_g = dr("final_g", [D], F32, kind="ExternalInput")
    out_h = dr("out", [NB * S, D], F32, kind="ExternalOutput")
    x_s = dr("x_s", [NB * S, D], F32)
    z_s = dr("z_s", [NB * CT, D], F32)
    modrows = dr("modrows", [3, 2, 6 * D], F32)
    dbg = {}

    P.phase_begin()
    cT = P.sb("cT", [128, 8, 3], F32)
    scT = P.sb("scT", [128, 8, 3], F32)
    rows = P.sb("rows", [3, 2, 6 * D], F32)
    adab = P.sb("adab", [3, 2, 6 * D], F32)
    gbc = P.sb("gbc", [3, 5, D], F32)
    wts = [P.sb("adaw%d" % i, [128, 8, 512], F32) for i in range(2)]
    pa = [P.ps("pa%d" % i, [128, 512], F32) for i in range(2)]
    P.dma("sp", cT[:], cT_in.ap(), writes=["cT"], ring="ld")
    for l in range(2):
        P.dma("act", adab[:, l, :], ada_b.ap()[l].partition_broadcast(3), writes=[("adab", l)], ring="ld")
    for i, src in enumerate((n1g.ap()[0], n1g.ap()[1], n2g.ap()[0], n2g.ap()[1], pool_scale.ap())):
        P.dma("act", gbc[:, i, :], src.partition_broadcast(3), writes=[("gbc", i)], ring="ld")
    P.op("act", lambda e: e.activation(scT[:], cT[:], AF.Silu), reads=["cT"], writes=["scT"])
    n = 0
    for l in range(2):
        for ft in range(12):
            b = n % 2
            n += 1
            P.dma("sp", wts[b][:], ada_w.ap()[l][:, ft * 512:(ft + 1) * 512].rearrange("(c p) f -> p c f", p=128),
                  writes=[("adaw", b)], ring="adaw", nring=2)
            for kc in range(8):
                P.op("pe", lambda e, b=b, kc=kc: e.matmul(pa[b][0:3, :], scT[:, kc, :], wts[b][:, kc, :],
                                                           start=(kc == 0), stop=(kc == 7)),
                     reads=["scT", ("adaw", b)], writes=[("pa", b)])
            P.op("dve", lambda e, b=b, l=l, ft=ft: e.tensor_tensor(
                rows[:, l, ft * 512:(ft + 1) * 512], pa[b][0:3, :], adab[:, l, ft * 512:(ft + 1) * 512], ALU.add),
                reads=[("pa", b), ("adab", l)], writes=[("rows", l, ft)])
    for l in range(2):
        allr = [("rows", l, ft) for ft in range(12)]
        P.op("dve", lambda e, l=l: e.scalar_tensor_tensor(rows[:, l, D:2 * D], rows[:, l, D:2 * D], 1.0, gbc[:, l, :], ALU.add, ALU.mult),
             reads=allr + [("gbc", l)], writes=allr)
        P.op("dve", lambda e, l=l: e.scalar_tensor_tensor(rows[:, l, 4 * D:5 * D], rows[:, l, 4 * D:5 * D], 1.0, gbc[:, 2 + l, :], ALU.add, ALU.mult),
             reads=allr + [("gbc", 2 + l)], writes=allr)
    allr0 = [("rows", 0, ft) for ft in range(12)]
    P.op("dve", lambda e: e.tensor_tensor(rows[:, 0, 2 * D:3 * D], rows[:, 0, 2 * D:3 * D], gbc[:, 4, :], ALU.mult),
         reads=allr0 + [("gbc", 4)], writes=allr0)
    P.dma("sp", modrows.ap(), rows[:], reads=[("rows", l, ft) for l in range(2) for ft in range(12)], writes=["modrows"], ring="st")
    P.phase_end()

    def load_bc(tile, stream, layer, reads_key="modrows"):
        for j in range(6):
            P.dma("act" if j % 2 else "sp", tile[:, j, :], modrows.ap()[stream, layer, j * D:(j + 1) * D].partition_broadcast(128),
                  writes=[("bc", j)], ring="ldbc", nring=6)

    def norm_mod(xt_ap, out_ap, bc, jg, jsh, sq, ssum, rstd, tmp, keys_r, key_w, tag, bcname="bc"):
        P.op("act", lambda e: e.activation(sq[:], xt_ap, AF.Square, accum_out=ssum[:, 0:1]),
             reads=keys_r, writes=[("ssum", tag), "sq"])
        P.op("act", lambda e: e.activation(rstd[:, 0:1], ssum[:, 0:1], AF.Sqrt, bias=EPS, scale=1.0 / D),
             reads=[("ssum", tag)], writes=[("rstd", tag)])
        P.op("dve", lambda e: e.reciprocal(rstd[:, 0:1], rstd[:, 0:1]), reads=[("rstd", tag)], writes=[("rstd", tag)])
        P.op("dve", lambda e: e.scalar_tensor_tensor(tmp[:], xt_ap, rstd[:, 0:1], bc[:, jg, :], ALU.mult, ALU.mult),
             reads=keys_r + [("rstd", tag), (bcname, jg)], writes=[("tmp", tag)])
        P.op("dve", lambda e: e.tensor_tensor(out_ap, tmp[:], bc[:, jsh, :], ALU.add),
             reads=[("tmp", tag), (bcname, jsh)], writes=[key_w])

    if upto >= 1:
        P.phase_begin()
        wdn = P.sb("wdn", [128, 22, D], BF16)
        pw = P.sb("pw", [128, 4, 2, 256], BF16)
        atx = P.sb("atx", [128, 4, 128], BF16)
        atz = P.sb("atz", [128, 4, 2, 256], BF16)
        identf = P.sb("identf", [128, 128], F32)
        ident = P.sb("ident", [128, 128], BF16)
        bcp = P.sb("bcp", [128, 5, D], F32)
        g2bc = [P.sb("g2bc%d" % i, [128, D], F32) for i in range(2)]
        xt2 = [P.sb("xt%d" % i, [128, 4, D], F32) for i in range(2)]
        hm = P.sb("hm", [128, 4, D], BF16)
        sq = P.sb("sq", [128, D], BF16)
        tmps = [P.sb("tmp%d" % i, [128, D], F32) for i in range(2)]
        ssum4 = P.sb("ssum4", [128, 4], F32)
        rstd4 = P.sb("rstd4", [128, 4], F32)
        ppT = P.sb("ppT", [128, 8, 128], BF16)
        hT2 = [P.sb("hT%d" % i, [128, 8, 512], BF16) for i in range(2)]
        act = P.sb("act", [128, 22, 512], BF16)
        sg = [P.sb("sg%d" % i, [128, 512], BF16) for i in range(2)]
        upds = [P.sb("upd%d" % i, [128, 512], F32) for i in range(2)]
        wgs = [P.sb("wgs%d" % i, [128, 2, 8, 256], BF16) for i in range(2)]
        pp = [P.ps("pp%d" % i, [128, 512], F32) for i in range(4)]
        py = [P.ps("py%d" % i, [128, 512], F32) for i in range(2)]
        pa0 = P.ps("pa0", [128, 512], F32)
        ptr = P.ps("ptr", [128, 8, 128], BF16)

        P.dma("pool", atz[:], atz_in.ap(), writes=["atz"], ring="ldw")
        P.dma("pool", atx[:], atx_in.ap(), writes=["atx"], ring="ldw")
        P.dma("pool", pw[:], pool_w.ap().rearrange("g (k p) n -> p g k n", p=128), writes=["pw"], ring="ldw")
        P.dma("pool", wdn[:], w_dn.ap().rearrange("(c p) n -> p c n", p=128), writes=["wdn"], ring="ldw")
        P.op("pool", lambda e: e.memset(identf[:], 0.0), writes=["identf"])
        P.op("pool", lambda e: e.affine_select(identf[:], identf[:], [[-1, 128]], ALU.not_equal, 1.0, base=0, channel_multiplier=1),
             reads=["identf"], writes=["identf"])
        P.op("dve", lambda e: e.tensor_copy(ident[:], identf[:]), reads=["identf"], writes=["ident"])

        groups = [(2, z_in, z_s, 0, True)]
        for b in range(NB):
            for gi in range(4):
                groups.append((b, x_in, x_s, b * S + gi * 512, False))
        NG = len(groups)
        BCJ = (0, 1, 2, 3, 4)
        xk = lambda gp, t: [("xt", gp, t), ("xt", gp, t, 0), ("xt", gp, t, 1)]

        def prep_steps(g):
            stream, src, dst, r0, is_ctx = groups[g]
            gp = g % 2
            xt = xt2[gp]
            hT = hT2[gp]
            new_stream = (g == 0) or (groups[g - 1][0] != stream)
            st = {}

            def m0():
                if new_stream:
                    for i, j in enumerate(BCJ):
                        P.dma("act" if i % 2 else "sp", bcp[:, i, :], modrows.ap()[stream, 0, j * D:(j + 1) * D].partition_broadcast(128),
                              writes=[("bcp", i)], ring="ldbc", nring=6)
                P.dma("act", g2bc[gp][:], modrows.ap()[stream, 0, 5 * D:6 * D].partition_broadcast(128), writes=[("g2bc", gp)], ring="ldbc", nring=6)
                P.dma("sp", xt[:], src.ap()[r0:r0 + 512, :].rearrange("(t p) d -> p t d", p=128),
                      writes=[k for t in range(4) for k in xk(gp, t)], ring="ldx", nring=2)

            def nrm4(jg, jsh, tag):
                for t in range(4):
                    P.op("act", lambda e, t=t: e.activation(sq[:], xt[:, t, :], AF.Square, accum_out=ssum4[:, t:t + 1]),
                         reads=xk(gp, t), writes=[("ssum4", t), "sq"])
                P.op("act", lambda e: e.activation(rstd4[:], ssum4[:], AF.Sqrt, bias=EPS, scale=1.0 / D),
                     reads=[("ssum4", t) for t in range(4)], writes=["rstd4"])
                P.op("dve", lambda e: e.reciprocal(rstd4[:], rstd4[:]), reads=["rstd4"], writes=["rstd4"])
                for t in range(4):
                    i2 = t % 2
                    P.op("dve", lambda e, t=t, i2=i2: e.scalar_tensor_tensor(tmps[i2][:], xt[:, t, :], rstd4[:, t:t + 1], bcp[:, jg, :], ALU.mult, ALU.mult),
                         reads=xk(gp, t) + ["rstd4", ("bcp", jg)], writes=[("tmp", i2)])
                    P.op("dve", lambda e, t=t, i2=i2: e.tensor_tensor(hm[:, t, :], tmps[i2][:], bcp[:, jsh, :], ALU.add),
                         reads=[("tmp", i2), ("bcp", jsh)], writes=[("hm", t)])

            def A(t):
                for cc in range(8):
                    gq = cc // 2
                    dstp = pa0 if cc < 4 else py[1]
                    dk = "pa0" if cc < 4 else ("py", 1)
                    o = dstp[:, (cc % 4) * 128:(cc % 4 + 1) * 128]
                    if not is_ctx:
                        P.op("pe", lambda e, o=o, t=t, cc=cc, gq=gq: e.matmul(o, hm[:, t, cc * 128:(cc + 1) * 128], atx[:, gq, :], start=True, stop=True),
                             reads=[("hm", t), "atx"], writes=[dk])
                    else:
                        zb, to = t // 2, t % 2
                        for ki in range(2):
                            P.op("pe", lambda e, o=o, zb=zb, ki=ki, cc=cc, gq=gq, to=to: e.matmul(
                                o, hm[:, zb * 2 + ki, cc * 128:(cc + 1) * 128], atz[:, gq, ki, to * 128:(to + 1) * 128],
                                start=(ki == 0), stop=(ki == 1)),
                                reads=[("hm", zb * 2 + ki), "atz"], writes=[dk])

            def C(t):
                P.op("act", lambda e: e.activation(ppT[:, 0:4, :].rearrange("p a b -> p (a b)"), pa0[:], AF.Copy), reads=["pa0"], writes=[("ppT", 0)])
                P.op("act", lambda e: e.activation(ppT[:, 4:8, :].rearrange("p a b -> p (a b)"), py[1][:], AF.Copy), reads=[("py", 1)], writes=[("ppT", 1)])

            def B(t):
                for gq in range(4):
                    dstp = py[0] if gq < 2 else pa0
                    dk = ("py", 0) if gq < 2 else "pa0"
                    for kc in range(2):
                        P.op("pe", lambda e, gq=gq, kc=kc, dstp=dstp: e.matmul(dstp[:, (gq % 2) * 256:(gq % 2 + 1) * 256], ppT[:, gq * 2 + kc, :], pw[:, gq, kc, :],
                                                                           start=(kc == 0), stop=(kc == 1)),
                             reads=[("ppT", gq // 2), "pw"], writes=[dk])

            def Ustep(t):
                for h in range(2):
                    srcp = py[0] if h == 0 else pa0
                    sk = ("py", 0) if h == 0 else "pa0"
                    P.op("dve", lambda e, h=h, srcp=srcp: e.tensor_tensor(upds[h][:], srcp[:], bcp[:, 2, h * 512:(h + 1) * 512], ALU.mult),
                         reads=[sk, ("bcp", 2)], writes=[("upd", h)])
                    P.op("dve", lambda e, h=h, t=t: e.tensor_tensor(xt[:, t, h * 512:(h + 1) * 512], xt[:, t, h * 512:(h + 1) * 512], upds[h][:], ALU.add),
                         reads=[("upd", h), ("xt", gp, t, h)], writes=[("xt", gp, t, h)])

            def T(t):
                for c in range(8):
                    P.op("pe", lambda e, t=t, c=c: e.transpose(ptr[:, c, :], hm[:, t, c * 128:(c + 1) * 128], ident[:]),
                         reads=[("hm", t), "ident"], writes=["ptr"])
                P.op("act", lambda e, t=t: e.activation(hT[:, :, t * 128:(t + 1) * 128], ptr[:], AF.Copy),
                     reads=["ptr"], writes=[("hT", gp, t)])

            sl = {i: [] for i in range(22)}
            sl[0] = [m0]
            sl[5] = [lambda: nrm4(1, 0, 0)]
            for t in range(4):
                sl[7 + 2 * t].append(lambda t=t: A(t))
                sl[8 + 2 * t].append(lambda t=t: C(t))
                sl[8 + 2 * t].append(lambda t=t: B(t))
                sl[8 + 2 * t].append(lambda t=t: Ustep(t))
            sl[15].append(lambda: nrm4(4, 3, 1))
            sl[20] += [lambda: T(0), lambda: T(1)]
            sl[21] += [lambda: T(2), lambda: T(3)]
            return sl

        wg_state = {"n": 0}

        def gateup_tile(g, ft):
            gp = g % 2
            hT = hT2[gp]
            hTk = [("hT", gp, t) for t in range(4)]
            if ft % 2 == 0:
                wb = wg_state["n"] % 2
                wg_state["n"] += 1
                wg_state["wb"] = wb
                P.dma("pool", wgs[wb][:].rearrange("p h c f -> p (h c f)"), w_gu.ap()[:, ft // 2, :],
                      writes=[("wgs", wb, 0), ("wgs", wb, 1)], ring="ldwg", nring=2)
            wb = wg_state["wb"]
            fi = ft % 2
            pb = (ft % 2) * 2
            for half in range(2):
                for kc in range(8):
                    P.op("pe", lambda e, wb=wb, half=half, kc=kc, fi=fi, pb=pb: e.matmul(
                        pp[pb + half][:], wgs[wb][:, half, kc, fi * 128:(fi + 1) * 128], hT[:, kc, :],
                        start=(kc == 0), stop=(kc == 7)),
                        reads=hTk + [("wgs", wb, half)], writes=[("pp", pb + half)])
            sgi = ft % 2
            P.op("act", lambda e, pb=pb, sgi=sgi: e.activation(sg[sgi][:], pp[pb][:], AF.Silu),
                 reads=[("pp", pb)], writes=[("sg", sgi)])
            P.op("dve", lambda e, pb=pb, sgi=sgi, ft=ft: e.tensor_tensor(act[:, ft, :], sg[sgi][:], pp[pb + 1][:], ALU.mult),
                 reads=[("sg", sgi), ("pp", pb + 1)], writes=[("act", ft)])

        def down(g):
            stream, src, dst, r0, is_ctx = groups[g]
            gp = g % 2
            xt = xt2[gp]
            actk = [("act", ft) for ft in range(22)]
            for t in range(4):
                for h in range(2):
                    for ft in range(22):
                        P.op("pe", lambda e, t=t, h=h, ft=ft: e.matmul(py[h][:], act[:, ft, t * 128:(t + 1) * 128], wdn[:, ft, h * 512:(h + 1) * 512],
                                                                       start=(ft == 0), stop=(ft == 21)),
                             reads=actk + ["wdn"], writes=[("py", h)])
                    P.op("dve", lambda e, h=h: e.tensor_tensor(upds[h][:], py[h][:], g2bc[gp][:, h * 512:(h + 1) * 512], ALU.mult),
                         reads=[("py", h), ("g2bc", gp)], writes=[("upd", h)])
                    P.op("dve", lambda e, h=h, t=t: e.tensor_tensor(xt[:, t, h * 512:(h + 1) * 512], xt[:, t, h * 512:(h + 1) * 512], upds[h][:], ALU.add),
                         reads=[("upd", h), ("xt", gp, t, h)], writes=[("xt", gp, t, h)])
            P.dma("sp", dst.ap()[r0:r0 + 512, :].rearrange("(t p) d -> p t d", p=128), xt[:],
                  reads=[k for t in range(4) for k in xk(gp, t)], writes=[("xs", id(dst), r0)], ring="stx", nring=2)

        sl0 = prep_steps(0)
        for i in range(22):
            for f_ in sl0[i]:
                f_()
        for g in range(NG):
            sln = prep_steps(g + 1) if g + 1 < NG else None
            for ft in range(22):
                gateup_tile(g, ft)
                if sln is not None:
                    for f_ in sln[ft]:
                        f_()
            down(g)
        P.phase_end()

    if upto >= 2:
        s_s = dr("s_s", [NB, 10, 128, S], BF16)
        TT = CT + S
        UP = 2 + CT + 1 + 2 + S + 1
        P.phase_begin()
        lpp = P.sb("lpp", [128, 10, 12], F32)
        hb = P.sb("hb", [128, 10, 4], F32)
        hcn = P.sb("hcn", [128, 10, 2], F32)
        hncn = P.sb("hncn", [128, 10, 2], F32)
        tl = [P.sb("tl%d" % i, [128, 10, 2], F32) for i in range(5)]
        wr = P.sb("wr", [128, 2, 10, 128], BF16)
        wi = P.sb("wi", [128, 2, 10, 128], BF16)
        win_u = [P.sb("winu%d" % i, [128, 8, 128], BF16) for i in range(2)]
        win_g = [P.sb("wing%d" % i, [128, 8, 128], BF16) for i in range(3)]
        identf = P.sb("identf", [128, 128], F32)
        ident = P.sb("ident", [128, 128], BF16)
        ssum = [P.sb("ssum%d" % i, [128, 1], F32) for i in range(4)]
        rstd = [P.sb("rstd%d" % i, [128, 1], F32) for i in range(4)]
        hT = P.sb("hT", [128, 8, TT], BF16)
        u_raw = [P.sb("u_raw%d" % i, [128, UP], F32) for i in range(2)]
        uc = [P.sb("uc%d" % i, [128, TT], F32) for i in range(2)]
        ucb = [P.sb("ucb%d" % i, [128, TT], BF16) for i in range(2)]
        gg = P.sb("gg", [128, S], BF16)
        Rb = [P.sb("Rb%d" % i, [128, TT], F32) for i in range(2)]
        Ib = [P.sb("Ib%d" % i, [128, TT], F32) for i in range(2)]
        Ab = [P.sb("Ab%d" % i, [128, TT], F32) for i in range(2)]
        Tb = [P.sb("Tb%d" % i, [128, TT], F32) for i in range(2)]
        Hb = [P.sb("Hb%d" % i, [128, TT], F32) for i in range(2)]
        sbf = P.sb("sbf", [128, S], BF16)
        pu = [P.ps("pu%d" % i, [128, 512], F32) for i in range(2)]
        pr = [P.ps("pr%d" % i, [128, 512], F32) for i in range(2)]
        pi = [P.ps("pi%d" % i, [128, 512], F32) for i in range(2)]
        ptr = [P.ps("ptr%d" % i, [128, 8, 128], BF16) for i in range(2)]
        K5 = lambda nm, d: [(nm, d, j) for j in range(5)]
        xts = [Rb[0][:, 0:D], Rb[1][:, 0:D], Tb[0][:, 0:D], Tb[1][:, 0:D]]
        xts_k = [K5("R", 0), K5("R", 1), K5("T", 0), K5("T", 1)]
        tmp = [Ib[i][:, 0:D] for i in range(2)]
        tmp_k = [K5("I", i) for i in range(2)]
        hm1 = [ucb[i][:, 0:D] for i in range(2)]
        hm1_k = [[("ucb", i, j) for j in range(5)] for i in range(2)]
        sq = gg[:, 0:D]
        sq_k = [("gg", j) for j in range(4)]
        bcz = Ab[0][:, 0:2 * D].rearrange("p (a b) -> p a b", a=2)
        bcz_k = K5("A", 0)
        bcx = Ab[1][:, 0:2 * D].rearrange("p (a b) -> p a b", a=2)
        bcx_k = K5("A", 1)

        P.dma("sp", lpp[:], lru_pp.ap(), writes=["lpp"], ring="ld")
        P.dma("pool", wr[:], lru_w_r.ap().rearrange("d h c n -> c d h n"), writes=["wr"], ring="ldw")
        P.dma("pool", wi[:], lru_w_i.ap().rearrange("d h c n -> c d h n"), writes=["wi"], ring="ldw")
        P.op("pool", lambda e: e.memset(identf[:], 0.0), writes=["identf"])
        P.op("pool", lambda e: e.affine_select(identf[:], identf[:], [[-1, 128]], ALU.not_equal, 1.0, base=0, channel_multiplier=1),
             reads=["identf"], writes=["identf"])
        P.op("dve", lambda e: e.tensor_copy(ident[:], identf[:]), reads=["identf"], writes=["ident"])
        for i in range(2):
            P.op("pool", lambda e, i=i: e.memset(u_raw[i][:], 0.0), writes=[("u_pad", i)])
        lam = lpp[:, :, 9:11]
        t0, t1, t2, t3, t4 = [t[:] for t in tl]
        P.op("act", lambda e: e.activation(t0, lam, AF.Abs), reads=["lpp"], writes=["t0"])
        P.op("act", lambda e: e.activation(t0, t0, AF.Exp, scale=-1.0), reads=["t0"], writes=["t0"])
        P.op("dve", lambda e: e.tensor_scalar(t1, t0, 2.0, None, ALU.add), reads=["t0"], writes=["t1"])
        P.op("dve", lambda e: e.reciprocal(t1, t1), reads=["t1"], writes=["t1"])
        P.op("dve", lambda e: e.tensor_tensor(t1, t1, t0, ALU.mult), reads=["t1", "t0"], writes=["t1"])
        P.op("dve", lambda e: e.tensor_tensor(t2, t1, t1, ALU.mult), reads=["t1"], writes=["t2"])
        P.op("dve", lambda e: e.tensor_scalar(t3, t2, 1.0 / 9.0, None, ALU.mult), reads=["t2"], writes=["t3"])
        for cst in (1.0 / 7.0, 1.0 / 5.0, 1.0 / 3.0):
            P.op("dve", lambda e, cst=cst: e.scalar_tensor_tensor(t3, t3, cst, t2, ALU.add, ALU.mult), reads=["t3", "t2"], writes=["t3"])
        P.op("dve", lambda e: e.scalar_tensor_tensor(t3, t3, 1.0, t1, ALU.add, ALU.mult), reads=["t3", "t1"], writes=["t3"])
        P.op("dve", lambda e: e.tensor_scalar(t4, lam, -1.0, 0.0, ALU.mult, ALU.max), reads=["lpp"], writes=["t4"])
        P.op("dve", lambda e: e.scalar_tensor_tensor(t4, t3, 2.0, t4, ALU.mult, ALU.add), reads=["t3", "t4"], writes=["t4"])
        P.op("dve", lambda e: e.tensor_scalar(hcn[:], t4, -4.0, None, ALU.mult), reads=["t4"], writes=["hcn"])
        P.op("dve", lambda e: e.tensor_scalar(hncn[:], t4, 4.0, None, ALU.mult), reads=["t4"], writes=["hncn"])
        P.op("dve", lambda e: e.tensor_scalar(hb[:], lpp[:, :, 5:9], 0.5, None, ALU.mult), reads=["lpp"], writes=["hb"])

        tiles = [(0, CT)] + [(CT + j * 512, 512) for j in range(4)]
        ucol = [2] + [(2 + CT + 1 + 2) + j * 512 for j in range(4)]
        hTk = [("hT", c) for c in range(0, TT, 128)]
        order = {0: [0, 1, 2, 3, 4], 1: [0, 4, 3, 2, 1]}
        state = {"xn": 0, "pn": 0}

        def build_hT(b):
            for j in range(2):
                P.dma("sp" if j == 0 else "act", bcz[:, j, :], modrows.ap()[2, 1, j * D:(j + 1) * D].partition_broadcast(128),
                      writes=bcz_k, ring="ldbc", nring=6)
                P.dma("act" if j == 0 else "sp", bcx[:, j, :], modrows.ap()[b, 1, j * D:(j + 1) * D].partition_broadcast(128),
                      writes=bcx_k, ring="ldbc", nring=6)
            srcs = [(z_s, b * CT + t * 128, bcz, bcz_k, t * 128) for t in range(2)] + \
                   [(x_s, b * S + t * 128, bcx, bcx_k, CT + t * 128) for t in range(16)]
            def front(src, r0, bct, bk, col0):
                xb = state["xn"] % 4
                hb_ = state["xn"] % 2
                state["xn"] += 1
                P.dma("sp" if xb % 2 == 0 else "act", xts[xb], src.ap()[r0:r0 + 128, :], writes=xts_k[xb], ring="ldx4", nring=4)
                P.op("act", lambda e, xb=xb: e.activation(sq, xts[xb], AF.Square, accum_out=ssum[xb][:, 0:1]),
                     reads=xts_k[xb], writes=sq_k + [("ssum", xb)])
                P.op("act", lambda e, xb=xb: e.activation(rstd[xb][:, 0:1], ssum[xb][:, 0:1], AF.Sqrt, bias=EPS, scale=1.0 / D),
                     reads=[("ssum", xb)], writes=[("rstd", xb)])
                P.op("dve", lambda e, xb=xb: e.reciprocal(rstd[xb][:, 0:1], rstd[xb][:, 0:1]), reads=[("rstd", xb)], writes=[("rstd", xb)])
                P.op("dve", lambda e, xb=xb, hb_=hb_, bct=bct: e.scalar_tensor_tensor(tmp[hb_], xts[xb], rstd[xb][:, 0:1], bct[:, 1, :], ALU.mult, ALU.mult),
                     reads=xts_k[xb] + [("rstd", xb)] + bk, writes=tmp_k[hb_])
                P.op("dve", lambda e, hb_=hb_, bct=bct: e.tensor_tensor(hm1[hb_], tmp[hb_], bct[:, 0, :], ALU.add),
                     reads=tmp_k[hb_] + bk, writes=hm1_k[hb_] + [("hm1", hb_)])
                for c in range(8):
                    P.op("pe", lambda e, c=c, hb_=hb_: e.transpose(ptr[hb_][:, c, :], hm1[hb_][:, c * 128:(c + 1) * 128], ident[:]),
                         reads=[("hm1", hb_), "ident"], writes=[("ptr", hb_)])
                return hb_

            def back(col0, hb_):
                P.op("act", lambda e, col0=col0, hb_=hb_: e.activation(hT[:, :, col0:col0 + 128], ptr[hb_][:], AF.Copy),
                     reads=[("ptr", hb_)], writes=[("hT", col0)])

            pend = None
            for (src, r0, bct, bk, col0) in srcs:
                hb_ = front(src, r0, bct, bk, col0)
                if pend is not None:
                    back(*pend)
                pend = (col0, hb_)
            back(*pend)

        def a1(b, h, par):
            wb = par
            gb = (b * 10 + h) % 3
            P.dma("pool", win_g[gb][:], lru_w_in.ap()[:, h * 128:(h + 1) * 128].rearrange("(c p) f -> p c f", p=128),
                  writes=[("wing", gb)], ring="ldwin0", nring=2)
            P.dma("pool", win_u[wb][:], lru_w_in.ap()[:, DR + h * 128:DR + (h + 1) * 128].rearrange("(c p) f -> p c f", p=128),
                  writes=[("winu", wb)], ring="ldwin1", nring=2)
            for j, (c0, n) in enumerate(tiles):
                pb = j % 2
                for kc in range(8):
                    P.op("pe", lambda e, pb=pb, kc=kc, c0=c0, n=n, wb=wb: e.matmul(pu[pb][:, 0:n], win_u[wb][:, kc, :], hT[:, kc, c0:c0 + n],
                                                                              start=(kc == 0), stop=(kc == 7)),
                         reads=hTk + [("winu", wb)], writes=[("pu", pb)])
                P.op("dve", lambda e, pb=pb, n=n, j=j, par=par: e.tensor_copy(u_raw[par][:, ucol[j]:ucol[j] + n], pu[pb][:, 0:n]),
                     reads=[("pu", pb), ("u_pad", par)], writes=[("u_raw", par, j)])

        def a2(b, h, par):
            for j, (c0, n) in enumerate(tiles):
                nb_ = [j] if j == 0 else [jj for jj in (j - 1, j, j + 1) if 1 <= jj <= 4]
                urk = [("u_raw", par, jj) for jj in nb_]
                r0 = ucol[j]
                P.op("act", lambda e, c0=c0, n=n, r0=r0, h=h, par=par: e.activation(uc[par][:, c0:c0 + n], u_raw[par][:, r0:r0 + n], AF.Identity,
                                                                                 bias=lpp[:, h, 4:5], scale=lpp[:, h, 2:3]),
                     reads=urk + ["lpp"], writes=[("uc", par, j)])

        def a3(b, h, par):
            for j, (c0, n) in enumerate(tiles):
                nb_ = [j] if j == 0 else [jj for jj in (j - 1, j, j + 1) if 1 <= jj <= 4]
                urk = [("u_raw", par, jj) for jj in nb_]
                r0 = ucol[j]
                for k in (0, 1, 3):
                    P.op("dve", lambda e, c0=c0, n=n, r0=r0, h=h, k=k, par=par: e.scalar_tensor_tensor(
                        uc[par][:, c0:c0 + n], u_raw[par][:, r0 - 2 + k:r0 - 2 + k + n], lpp[:, h, k:k + 1], uc[par][:, c0:c0 + n], ALU.mult, ALU.add),
                        reads=urk + ["lpp", ("uc", par, j)], writes=[("uc", par, j)])

        def a4(b, h, par):
            for j, (c0, n) in enumerate(tiles):
                P.op("act", lambda e, c0=c0, n=n, par=par: e.activation(ucb[par][:, c0:c0 + n], uc[par][:, c0:c0 + n], AF.Copy),
                     reads=[("uc", par, j)], writes=[("ucb", par, j)])

        def b1(b, h, par):
            for d in range(2):
                for j in order[d]:
                    c0, n = tiles[j]
                    pb = state["pn"] % 2
                    state["pn"] += 1
                    P.op("pe", lambda e, pb=pb, c0=c0, n=n, d=d: e.matmul(pr[pb][:, 0:n], wr[:, d, h, :], ucb[par][:, c0:c0 + n], start=True, stop=True),
                         reads=[("ucb", par, j), "wr"], writes=[("pr", pb)])
                    P.op("pe", lambda e, pb=pb, c0=c0, n=n, d=d: e.matmul(pi[pb][:, 0:n], wi[:, d, h, :], ucb[par][:, c0:c0 + n], start=True, stop=True),
                         reads=[("ucb", par, j), "wi"], writes=[("pi", pb)])
                    P.op("act", lambda e, pb=pb, c0=c0, n=n, d=d: e.activation(Rb[d][:, c0:c0 + n], pr[pb][:, 0:n], AF.Tanh,
                                                                          bias=hb[:, h, d:d + 1], scale=0.5),
                         reads=[("pr", pb), "hb"], writes=[("R", d, j)])
                    P.op("act", lambda e, pb=pb, c0=c0, n=n, d=d: e.activation(Ib[d][:, c0:c0 + n], pi[pb][:, 0:n], AF.Tanh,
                                                                          bias=hb[:, h, 2 + d:3 + d], scale=0.5),
                         reads=[("pi", pb), "hb"], writes=[("I", d, j)])

        def b2(b, h, par):
            for d in range(2):
                for j in order[d]:
                    c0, n = tiles[j]
                    sl = slice(c0, c0 + n)
                    P.op("act", lambda e, d=d, sl=sl: e.activation(Ab[d][:, sl], Rb[d][:, sl], AF.Exp, bias=hcn[:, h, d:d + 1], scale=hcn[:, h, d:d + 1]),
                         reads=[("R", d, j), "hcn"], writes=[("A", d, j)])
                    P.op("act", lambda e, d=d, sl=sl: e.activation(Tb[d][:, sl], Rb[d][:, sl], AF.Tanh, bias=hncn[:, h, d:d + 1], scale=hncn[:, h, d:d + 1]),
                         reads=[("R", d, j), "hncn"], writes=[("T", d, j)])
                    P.op("act", lambda e, d=d, sl=sl: e.activation(Rb[d][:, sl], Ab[d][:, sl], AF.Square),
                         reads=[("A", d, j)], writes=[("R", d, j)])
                    P.op("dve", lambda e, d=d, sl=sl: e.scalar_tensor_tensor(Tb[d][:, sl], Rb[d][:, sl], 1.0, Tb[d][:, sl], ALU.add, ALU.mult),
                         reads=[("R", d, j), ("T", d, j)], writes=[("T", d, j)])
                    P.op("dve", lambda e, d=d, sl=sl: e.scalar_tensor_tensor(Ib[d][:, sl], Ib[d][:, sl], 1.0, uc[par][:, sl], ALU.add, ALU.mult),
                         reads=[("I", d, j), ("uc", par, j)], writes=[("I", d, j)])

        def b3a(b, h, par):
            for d in range(2):
                for j in order[d]:
                    c0, n = tiles[j]
                    sl = slice(c0, c0 + n)
                    P.op("act", lambda e, d=d, sl=sl: e.activation(Tb[d][:, sl], Tb[d][:, sl], AF.Sqrt, scale=0.25),
                         reads=[("T", d, j)], writes=[("T", d, j)])

        def b3b(b, h, par):
            for d in range(2):
                prev = None
                for j in order[d]:
                    c0, n = tiles[j]
                    sl = slice(c0, c0 + n)
                    P.op("dve", lambda e, d=d, sl=sl: e.tensor_tensor(Tb[d][:, sl], Tb[d][:, sl], Ib[d][:, sl], ALU.mult),
                         reads=[("T", d, j), ("I", d, j)], writes=[("T", d, j)])
                    if d == 0:
                        init = 0.0 if prev is None else Hb[0][:, c0 - 1:c0]
                        P.op("dve", lambda e, sl=sl, init=init: e.tensor_tensor_scan(Hb[0][:, sl], Ab[0][:, sl], Tb[0][:, sl], init, ALU.mult, ALU.add),
                             reads=[("A", 0, j), ("T", 0, j)] + ([("H", 0, prev)] if prev is not None else []), writes=[("H", 0, j)])
                    else:
                        rs = slice(c0 + n - 1, (c0 - 1) if c0 > 0 else None, -1)
                        if prev is None:
                            init = 0.0
                        elif prev == 0:
                            init = Hb[1][:, 0:1]
                        else:
                            init = Hb[1][:, tiles[prev][0]:tiles[prev][0] + 1]
                        P.op("dve", lambda e, rs=rs, init=init: e.tensor_tensor_scan(Hb[1][:, rs], Ab[1][:, rs], Tb[1][:, rs], init, ALU.mult, ALU.add),
                             reads=[("A", 1, j), ("T", 1, j)] + ([("H", 1, prev)] if prev is not None else []), writes=[("H", 1, j)])
                    prev = j

        def b4(b, h, par):
            gb = (b * 10 + h) % 3
            for j in range(4):
                pb = j % 2
                c0 = CT + j * 512
                for kc in range(8):
                    P.op("pe", lambda e, pb=pb, kc=kc, c0=c0, gb=gb: e.matmul(pu[pb][:], win_g[gb][:, kc, :], hT[:, kc, c0:c0 + 512],
                                                                         start=(kc == 0), stop=(kc == 7)),
                         reads=hTk + [("wing", gb)], writes=[("pu", pb)])
                P.op("act", lambda e, pb=pb, j=j: e.activation(gg[:, j * 512:(j + 1) * 512], pu[pb][:], AF.Gelu),
                     reads=[("pu", pb)], writes=[("gg", j)])
            for j in range(1, 5):
                c0, n = tiles[j]
                sl = slice(c0, c0 + n)
                P.op("dve", lambda e, sl=sl: e.tensor_tensor(Hb[0][:, sl], Hb[0][:, sl], Hb[1][:, sl], ALU.add),
                     reads=[("H", 0, j), ("H", 1, j)], writes=[("H", 0, j)])
                P.op("dve", lambda e, sl=sl, j=j: e.tensor_tensor(sbf[:, (j - 1) * 512:j * 512], Hb[0][:, sl], gg[:, (j - 1) * 512:j * 512], ALU.mult),
                     reads=[("H", 0, j), ("gg", j - 1)], writes=[("sbf", j)])
            P.dma("sp", s_s.ap()[b, h], sbf[:], reads=[("sbf", j) for j in range(1, 5)], writes=[("sbf", j) for j in range(1, 5)], ring="sts", nring=2)

        for b in range(NB):
            build_hT(b)
            for f_ in (a1, a2, a3, a4):
                f_(b, 0, 0)
            for h in range(10):
                nxt = h + 1 < 10
                np_ = (h + 1) % 2
                b1(b, h, h % 2)
                if nxt:
                    a1(b, h + 1, np_)
                b2(b, h, h % 2)
                if nxt:
                    a2(b, h + 1, np_)
                b3a(b, h, h % 2)
                if nxt:
                    a3(b, h + 1, np_)
                b3b(b, h, h % 2)
                if nxt:
                    a4(b, h + 1, np_)
                b4(b, h, h % 2)
        P.phase_end()

        P.phase_begin()
        wout = P.sb("wout", [128, 10, D], BF16)
        sT2 = [P.sb("sT%d" % i, [128, 10, S], BF16) for i in range(2)]
        bcg2b = [P.sb("bcg%d" % i, [128, D], F32) for i in range(2)]
        xts = [P.sb("xts%d" % i, [128, D], F32) for i in range(3)]
        upd = P.sb("upd", [128, 512], F32)
        py = [P.ps("py%d" % i, [128, 512], F32) for i in range(4)]
        P.dma("pool", wout[:], lru_w_out.ap().rearrange("(c p) n -> p c n", p=128), writes=["wout"], ring="ldw")
        xn = 0
        for b in range(NB):
            P.dma("sp" if b == 0 else "act", sT2[b][:], s_s.ap()[b].rearrange("h p t -> p h t"), writes=[("sT", b)], ring="ldsT")
            P.dma("act", bcg2b[b][:], modrows.ap()[b, 1, 2 * D:3 * D].partition_broadcast(128), writes=[("bcg", b)], ring="ldbc", nring=6)
        for b in range(NB):
            sT = sT2[b]
            bcg = bcg2b[b]
            for t in range(16):
                xb = xn % 3
                r0 = b * S + t * 128
                P.dma("sp", xts[xb][:], x_s.ap()[r0:r0 + 128, :], writes=[("xts", xb)], ring="ldx", nring=2)
                for hh in range(2):
                    pb = (xn % 2) * 2 + hh
                    for h in range(10):
                        P.op("pe", lambda e, pb=pb, h=h, t=t, hh=hh, sT=sT: e.matmul(py[pb][:], sT[:, h, t * 128:(t + 1) * 128], wout[:, h, hh * 512:(hh + 1) * 512],
                                                                        start=(h == 0), stop=(h == 9)),
                             reads=[("sT", b), "wout"], writes=[("py", pb)])
                    P.op("dve", lambda e, pb=pb, hh=hh, bcg=bcg: e.tensor_tensor(upd[:], py[pb][:], bcg[:, hh * 512:(hh + 1) * 512], ALU.mult),
                         reads=[("py", pb), ("bcg", b)], writes=["upd"])
                    P.op("dve", lambda e, xb=xb, hh=hh: e.tensor_tensor(xts[xb][:, hh * 512:(hh + 1) * 512], xts[xb][:, hh * 512:(hh + 1) * 512], upd[:], ALU.add),
                         reads=["upd", ("xts", xb)], writes=[("xts", xb)])
                xn += 1
                P.dma("act", x_s.ap()[r0:r0 + 128, :], xts[xb][:], reads=[("xts", xb)], ring="stx", nring=2)
        P.phase_end()

    if upto >= 3:
        NJ = 8
        CW = 2 * DE // NJ
        xs_h = dr("xs_h", [NSLOT, D], BF16)
        y_h = dr("y_h", [NSLOT, D], F32)
        idxgu_s = dr("idxgu_s", [128, NBLK * 8], I32)
        idxdn_s = dr("idxdn_s", [128, NBLK * 4], I32)
        pos_s = dr("pos_s", [128, 64], I32)
        w_s = dr("w_s", [128, 64], F32)
        NTT = NB * S // 128
        P.phase_begin()
        mc = P.sb("mc", [128, 632], F32)
        U = mc[:, 0:128]
        ones = mc[:, 128:256]
        cgu = mc[:, 256:264]
        cdn = mc[:, 320:324]
        thr = mc[:, 348:540].rearrange("p (b e) -> p b e", e=8)
        k8192 = mc[:, 540:548]
        k3584 = mc[:, 604:608]
        identf = P.sb("identf", [128, 128], F32)
        wrt = P.sb("wrt", [128, 8, NE], F32)
        bc2 = P.sb("bc2", [128, 2, D], F32)
        xts = [P.sb("xts%d" % i, [128, D], F32) for i in range(2)]
        sq = P.sb("sq", [128, D], BF16)
        tmp = P.sb("tmp", [128, D], F32)
        hm32 = P.sb("hm32", [128, D], F32)
        ssum = P.sb("ssum", [128, 1], F32)
        rstd = P.sb("rstd", [128, 1], F32)
        hmb = P.sb("hmb", [128, NTT, D], BF16)
        hmT32 = P.sb("hmT32", [128, 8, 128], F32)
        lg = P.sb("lg", [128, 8], F32)
        mx8 = P.sb("mx8", [128, 8], F32)
        negm2 = P.sb("negm2", [128, 1], F32)
        M_all = P.sb("M_all", [128, NTT, 8], F32)
        M1_all = P.sb("M1_all", [128, NTT, 8], F32)
        M2_all = P.sb("M2_all", [128, NTT, 8], F32)
        W_all = P.sb("W_all", [128, NTT, 2], F32)
        Rs = P.sb("Rs", [128, NTT, 8], F32)
        Tt = P.sb("Tt", [128, NTT, 8], F32)
        Cin = P.sb("Cin", [128, NTT, 8], F32)
        dest = P.sb("dest", [128, NTT, 8], F32)
        cnt_i = P.sb("cnt_i", [128, 8], I32)
        padf = P.sb("padf", [128, 8], F32)
        pend = P.sb("pend", [128, 8], F32)
        pstart = P.sb("pstart", [128, 8], F32)
        pos_f = P.sb("pos_f", [128, NTT, 2], F32)
        pos_i = P.sb("pos_i", [128, NTT, 2], I32)
        cmp = P.sb("cmp", [128, NBLK, 8], F32)
        be = P.sb("be", [128, NBLK], F32)
        idxgu_f = P.sb("idxgu_f", [128, NBLK, 8], F32)
        idxgu_i = P.sb("idxgu_i", [128, NBLK, 8], I32)
        idxdn_f = P.sb("idxdn_f", [128, NBLK, 4], F32)
        idxdn_i = P.sb("idxdn_i", [128, NBLK, 4], I32)
        ptf = [P.ps("ptf%d" % i, [128, 4, 128], F32) for i in range(2)]
        plg = P.ps("plg", [128, 512], F32)
        pR = P.ps("pR", [128, 512], F32)
        pT = P.ps("pT", [128, 512], F32)

        P.dma("sp", mc[:], mconst.ap(), writes=["mc"], ring="ld")
        P.dma("sp", wrt[:], w_router.ap().rearrange("(c p) e -> p c e", p=128), writes=["wrt"], ring="ld")
        P.op("pool", lambda e: e.memset(identf[:], 0.0), writes=["identf"])
        P.op("pool", lambda e: e.affine_select(identf[:], identf[:], [[-1, 128]], ALU.not_equal, 1.0, base=0, channel_multiplier=1),
             reads=["identf"], writes=["identf"])
        zt = P.sb("zt", [128, 4 * D], BF16)
        P.op("pool", lambda e: e.memset(zt[:], 0.0), writes=["zt"])
        for blk in range(NBLK):
            P.dma("pool", xs_h.ap()[blk * BLK:(blk + 1) * BLK, :].rearrange("(p t) d -> p (t d)", p=128), zt[:], reads=["zt"],
                  writes=[("xsz", blk)], ring="stz", nring=4)
        tmp2 = [tmp, P.sb("tmpB", [128, D], F32)]
        hm32_2 = [hm32, P.sb("hm32B", [128, D], F32)]
        ssum2 = [ssum, P.sb("ssumB", [128, 1], F32)]
        rstd2 = [rstd, P.sb("rstdB", [128, 1], F32)]
        hmT32_2 = [hmT32, P.sb("hmT32B", [128, 8, 128], F32)]
        lg2 = [lg, P.sb("lgB", [128, 8], F32)]
        mx82 = [mx8, P.sb("mx8B", [128, 8], F32)]
        negm22 = [negm2, P.sb("negm2B", [128, 1], F32)]
        ptf4 = ptf + [P.ps("ptf%d" % i, [128, 4, 128], F32) for i in range(2, 4)]
        plg2 = [plg, P.ps("plgB", [128, 512], F32)]
        tmp2.append(P.sb("tmpC", [128, D], F32)); hm32_2.append(P.sb("hm32C", [128, D], F32))
        ssum2.append(P.sb("ssumC", [128, 1], F32)); rstd2.append(P.sb("rstdC", [128, 1], F32))
        hmT32_2.append(P.sb("hmT32C", [128, 8, 128], F32)); lg2.append(P.sb("lgC", [128, 8], F32)); mx82.append(P.sb("mx8C", [128, 8], F32))
        xts3 = xts + [P.sb("xtsC", [128, D], F32)]
        sq3 = [sq, P.sb("sqB", [128, D], BF16), P.sb("sqC", [128, D], BF16)]

        bc2s = [bc2, P.sb("bc2B", [128, 2, D], F32)]
        for b in range(NB):
            for q in range(2):
                P.dma("sp" if q == 0 else "act", bc2s[b][:, q, :], modrows.ap()[b, 1, (3 + q) * D:(4 + q) * D].partition_broadcast(128),
                      writes=[("bc2", b, q)], ring="ldbc", nring=6)

        def s1(j):
            b = j // 16
            bc2 = bc2s[b]
            x3 = j % 3
            tmp_, hm_, ss_, rs_ = tmp2[x3], hm32_2[x3], ssum2[x3], rstd2[x3]
            P.dma("sp", xts3[x3][:], x_s.ap()[j * 128:(j + 1) * 128, :], writes=[("xts", x3)], ring="ldx3", nring=3)
            P.op("act", lambda e, x3=x3, ss_=ss_: e.activation(sq3[x3][:], xts3[x3][:], AF.Square, accum_out=ss_[:, 0:1]), reads=[("xts", x3)], writes=[("ssum", x3), ("sq", x3)])
            P.op("act", lambda e, ss_=ss_, rs_=rs_: e.activation(rs_[:, 0:1], ss_[:, 0:1], AF.Sqrt, bias=EPS, scale=1.0 / D), reads=[("ssum", x3)], writes=[("rstd", x3)])
            P.op("dve", lambda e, rs_=rs_: e.reciprocal(rs_[:, 0:1], rs_[:, 0:1]), reads=[("rstd", x3)], writes=[("rstd", x3)])
            P.op("dve", lambda e, x3=x3, rs_=rs_, tmp_=tmp_, bc2=bc2: e.scalar_tensor_tensor(tmp_[:], xts3[x3][:], rs_[:, 0:1], bc2[:, 1, :], ALU.mult, ALU.mult),
                 reads=[("xts", x3), ("rstd", x3), ("bc2", b, 1)], writes=[("tmp", x3)])
            P.op("dve", lambda e, tmp_=tmp_, hm_=hm_, bc2=bc2: e.tensor_tensor(hm_[:], tmp_[:], bc2[:, 0, :], ALU.add), reads=[("tmp", x3), ("bc2", b, 0)], writes=[("hm32", x3)])

        def s2(j):
            x3 = j % 3
            xb = j % 2
            hm_, hT_, pl_ = hm32_2[x3], hmT32_2[x3], plg2[xb]
            P.op("act", lambda e, j=j, hm_=hm_: e.activation(hmb[:, j, :], hm_[:], AF.Copy), reads=[("hm32", x3)], writes=[("hmb", j)])
            for c in range(8):
                P.op("pe", lambda e, c=c, hm_=hm_, xb=xb: e.transpose(ptf4[xb * 2 + c // 4][:, c % 4, :], hm_[:, c * 128:(c + 1) * 128], identf[:]),
                     reads=[("hm32", x3), "identf"], writes=[("ptf", xb * 2 + c // 4)])
            for h2 in range(2):
                P.op("dve" if h2 == 0 else "act",
                     (lambda e, h2=h2, hT_=hT_, xb=xb: e.tensor_copy(hT_[:, h2 * 4:(h2 + 1) * 4, :], ptf4[xb * 2 + h2][:])) if h2 == 0 else
                     (lambda e, h2=h2, hT_=hT_, xb=xb: e.activation(hT_[:, h2 * 4:(h2 + 1) * 4, :], ptf4[xb * 2 + h2][:], AF.Copy)),
                     reads=[("ptf", xb * 2 + h2)], writes=[("hmT32", x3, h2)])
            for c in range(8):
                P.op("pe", lambda e, c=c, hT_=hT_, pl_=pl_: e.matmul(pl_[:, 0:8], hT_[:, c, :], wrt[:, c, :], start=(c == 0), stop=(c == 7)),
                     reads=[("hmT32", x3, c // 4), "wrt"], writes=[("plg", xb)])

        def s3(j):
            x3 = j % 3
            xb = j % 2
            lg_, mx_, pl_ = lg2[x3], mx82[x3], plg2[xb]
            P.op("act", lambda e, lg_=lg_, pl_=pl_: e.activation(lg_[:], pl_[:, 0:8], AF.Copy), reads=[("plg", xb)], writes=[("lg", x3)])
            P.op("dve", lambda e, lg_=lg_, mx_=mx_: e.max(mx_[:], lg_[:]), reads=[("lg", x3)], writes=[("mx8", x3)])
            P.op("dve", lambda e, j=j, lg_=lg_, mx_=mx_: e.tensor_scalar(M1_all[:, j, :], lg_[:], mx_[:, 0:1], None, ALU.is_equal), reads=[("lg", x3), ("mx8", x3)], writes=[("M1", j)])
            P.op("dve", lambda e, j=j, lg_=lg_, mx_=mx_: e.tensor_scalar(M2_all[:, j, :], lg_[:], mx_[:, 1:2], None, ALU.is_equal), reads=[("lg", x3), ("mx8", x3)], writes=[("M2", j)])
            P.op("dve", lambda e, j=j, mx_=mx_: e.tensor_tensor(W_all[:, j, 0:1], mx_[:, 0:1], mx_[:, 1:2], ALU.subtract), reads=[("mx8", x3)], writes=[("W", j)])

        for it in range(NTT + 2):
            if it < NTT:
                s1(it)
            if 0 <= it - 1 < NTT:
                s2(it - 1)
            if 0 <= it - 2 < NTT:
                s3(it - 2)
        allW = [("W", j) for j in range(NTT)]
        P.op("act", lambda e: e.activation(W_all[:, :, 0], W_all[:, :, 0], AF.Sigmoid), reads=allW, writes=allW)
        P.op("dve", lambda e: e.tensor_scalar(W_all[:, :, 1], W_all[:, :, 0], -1.0, 1.0, ALU.mult, ALU.add), reads=allW, writes=allW)
        allM1 = [("M1", j) for j in range(NTT)]
        allM2 = [("M2", j) for j in range(NTT)]
        fl = lambda t: t[:].rearrange("p j e -> p (j e)")
        P.op("dve", lambda e: e.tensor_tensor(fl(M_all), fl(M1_all), fl(M2_all), ALU.add), reads=allM1 + allM2, writes=["M"])
        P.op("pe", lambda e: e.matmul(pR[:, 0:256], U, fl(M_all), start=True, stop=True), reads=["M", "mc"], writes=["pR"])
        P.op("pe", lambda e: e.matmul(pT[:, 0:256], ones, fl(M_all), start=True, stop=True), reads=["M", "mc"], writes=["pT"])
        P.op("act", lambda e: e.activation(fl(Rs), pR[:, 0:256], AF.Copy), reads=["pR"], writes=["Rs"])
        P.op("dve", lambda e: e.tensor_copy(fl(Tt), pT[:, 0:256]), reads=["pT"], writes=["Tt"])
        for ee in range(8):
            P.op("dve", lambda e, ee=ee: e.tensor_tensor_scan(Cin[:, :, ee], ones[:, 0:NTT], Tt[:, :, ee], 0.0, ALU.mult, ALU.add),
                 reads=["Tt", "mc"], writes=["Cin"])
        P.op("dve", lambda e: e.tensor_copy(cnt_i[:], Cin[:, NTT - 1, :]), reads=["Cin"], writes=["cnt_i"])
        P.op("dve", lambda e: e.tensor_scalar(cnt_i[:], cnt_i[:], BLK - 1, None, ALU.add), reads=["cnt_i"], writes=["cnt_i"])
        P.op("dve", lambda e: e.tensor_scalar(cnt_i[:], cnt_i[:], 9, 9, ALU.arith_shift_right, ALU.logical_shift_left), reads=["cnt_i"], writes=["cnt_i"])
        P.op("dve", lambda e: e.tensor_copy(padf[:], cnt_i[:]), reads=["cnt_i"], writes=["padf"])
        P.op("dve", lambda e: e.tensor_tensor_scan(pend[:], ones[:, 0:8], padf[:], 0.0, ALU.mult, ALU.add), reads=["padf", "mc"], writes=["pend"])
        P.op("dve", lambda e: e.tensor_tensor(pstart[:], pend[:], padf[:], ALU.subtract), reads=["pend", "padf"], writes=["pstart"])
        P.op("dve", lambda e: e.tensor_tensor(fl(dest), fl(Cin), fl(Tt), ALU.subtract), reads=["Cin", "Tt"], writes=["dest"])
        P.op("dve", lambda e: e.tensor_tensor(fl(dest), fl(dest), fl(Rs), ALU.add), reads=["dest", "Rs"], writes=["dest"])
        P.op("dve", lambda e: e.tensor_tensor(dest[:], dest[:], pstart[:].unsqueeze(1).to_broadcast([128, NTT, 8]), ALU.add), reads=["dest", "pstart"], writes=["dest"])
        P.op("dve", lambda e: e.tensor_tensor(fl(M1_all), fl(M1_all), fl(dest), ALU.mult), reads=allM1 + ["dest", "M"], writes=allM1)
        P.op("dve", lambda e: e.tensor_tensor(fl(M2_all), fl(M2_all), fl(dest), ALU.mult), reads=allM2 + ["dest", "M"], writes=allM2)
        P.op("dve", lambda e: e.tensor_reduce(pos_f[:, :, 0], M1_all[:], AX.X, ALU.add), reads=allM1, writes=["pos_f0"])
        P.op("dve", lambda e: e.tensor_reduce(pos_f[:, :, 1], M2_all[:], AX.X, ALU.add), reads=allM2, writes=["pos_f1"])
        P.op("dve", lambda e: e.tensor_copy(pos_i[:], pos_f[:]), reads=["pos_f0", "pos_f1"], writes=["pos_i"])
        P.op("dve", lambda e: e.tensor_tensor(cmp[:], thr, pend[:].unsqueeze(1).to_broadcast([128, NBLK, 8]), ALU.is_ge), reads=["mc", "pend"], writes=["cmp"])
        P.op("dve", lambda e: e.tensor_reduce(be[:], cmp[:], AX.X, ALU.add), reads=["cmp"], writes=["be"])
        P.op("dve", lambda e: e.tensor_scalar(be[:], be[:], float(NE - 1), None, ALU.min), reads=["be"], writes=["be"])
        for blk in range(NBLK):
            P.op("dve", lambda e, blk=blk: e.scalar_tensor_tensor(idxgu_f[:, blk, :], k8192, be[:, blk:blk + 1], cgu, ALU.mult, ALU.add),
                 reads=["be", "mc"], writes=["idxgu_f"])
            P.op("dve", lambda e, blk=blk: e.scalar_tensor_tensor(idxdn_f[:, blk, :], k3584, be[:, blk:blk + 1], cdn, ALU.mult, ALU.add),
                 reads=["be", "mc"], writes=["idxdn_f"])
        P.op("dve", lambda e: e.tensor_copy(idxgu_i[:], idxgu_f[:]), reads=["idxgu_f"], writes=["idxgu_i"])
        P.op("dve", lambda e: e.tensor_copy(idxdn_i[:], idxdn_f[:]), reads=["idxdn_f"], writes=["idxdn_i"])
        P.dma("sp", idxgu_s.ap(), idxgu_i[:].rearrange("p b c -> p (b c)"), reads=["idxgu_i"], ring="st")
        P.dma("sp", idxdn_s.ap(), idxdn_i[:].rearrange("p b c -> p (b c)"), reads=["idxdn_i"], ring="st")
        P.dma("sp", pos_s.ap(), pos_i[:].rearrange("p j k -> p (j k)"), reads=["pos_i"], ring="st")
        P.dma("sp", w_s.ap(), W_all[:].rearrange("p j k -> p (j k)"), reads=[("W", j) for j in range(NTT)], ring="st")
        for j in range(NTT):
            for k in range(2):
                P.scatter(xs_h.ap()[:, :], hmb[:, j, :], pos_i[:, j, k:k + 1], reads=[("hmb", j), "pos_i"] + [("xsz", blk) for blk in range(NBLK)], writes=[("xs", j, k)])
        if debug:
            dlg = dr("dbg_be", [128, NBLK], F32, kind="ExternalOutput")
            P.dma("sp", dlg.ap(), be[:], reads=["be"], ring="st")
            dps = dr("dbg_pos", [128, 64], F32, kind="ExternalOutput")
            P.dma("sp", dps.ap(), pos_f[:].rearrange("p j k -> p (j k)"), reads=["pos_f0", "pos_f1"], ring="st")
            dpe = dr("dbg_pend", [128, 8], F32, kind="ExternalOutput")
            P.dma("sp", dpe.ap(), pend[:], reads=["pend"], ring="st")
        P.phase_end()

        P.phase_begin()
        idxgu = P.sb("idxgu", [128, NBLK, 8], I32)
        idxdn = P.sb("idxdn", [128, NBLK, 4], I32)
        identf = P.sb("identf", [128, 128], F32)
        ident = P.sb("ident", [128, 128], BF16)
        xrs = [P.sb("xr%d" % i, [128, 4, D], BF16) for i in range(2)]
        xsT = P.sb("xsT", [128, 8, 512], BF16)
        wg = [P.sb("wg%d" % i, [128, 2, 4, 2 * CW], BF16) for i in range(3)]
        wd = P.sb("wd", [128, 4, 7, D], BF16)
        act = P.sb("act", [128, 28, 512], BF16)
        sg = [P.sb("sg%d" % i, [128, 512], BF16) for i in range(2)]
        yt = [P.sb("yt%d" % i, [128, D], F32) for i in range(2)]
        pp = [P.ps("pp%d" % i, [128, 512], F32) for i in range(4)]
        py = [P.ps("py%d" % i, [128, 512], F32) for i in range(2)]
        ptrs = [P.ps("ptr%d" % i, [128, 8, 128], BF16) for i in range(2)]
        P.dma("sp", idxgu[:].rearrange("p b c -> p (b c)"), idxgu_s.ap(), writes=["idxgu"], ring="ld")
        P.dma("sp", idxdn[:].rearrange("p b c -> p (b c)"), idxdn_s.ap(), writes=["idxdn"], ring="ld")
        P.op("pool", lambda e: e.memset(identf[:], 0.0), writes=["identf"])
        P.op("pool", lambda e: e.affine_select(identf[:], identf[:], [[-1, 128]], ALU.not_equal, 1.0, base=0, channel_multiplier=1),
             reads=["identf"], writes=["identf"])
        P.op("dve", lambda e: e.tensor_copy(ident[:], identf[:]), reads=["identf"], writes=["ident"])
        wgn = 0
        ytn = 0

        def gu_stage_load(blk, s, wb):
            for h2 in range(2):
                col = s * 2 + h2
                P.gather(wg[wb][:, h2, :, :].rearrange("p k c -> p (k c)"), moe_gu.ap()[:, :], idxgu[:, blk, col:col + 1],
                         reads=["idxgu"], writes=[("wg", wb, h2)], ring="ig", nring=4)

        def xr_load(blk):
            P.dma("sp", xrs[blk % 2][:], xs_h.ap()[blk * BLK:(blk + 1) * BLK, :].rearrange("(t p) d -> p t d", p=128),
                  writes=[("xr", blk % 2)], ring="ldx", nring=2)

        xr_load(0)
        NST = NBLK * 4
        for n0 in range(2):
            gu_stage_load(n0 // 4, n0 % 4, n0 % 3)
        def do_transposes(blk):
            xr = xrs[blk % 2]
            for t in range(4):
                ptr = ptrs[t % 2]
                for c in range(8):
                    P.op("pe", lambda e, t=t, c=c, xr=xr, ptr=ptr: e.transpose(ptr[:, c, :], xr[:, t, c * 128:(c + 1) * 128], ident[:]),
                         reads=[("xr", blk % 2), "ident"], writes=[("ptr", t % 2)])
                P.op("act", lambda e, t=t, ptr=ptr: e.activation(xsT[:, :, t * 128:(t + 1) * 128], ptr[:], AF.Copy), reads=[("ptr", t % 2)], writes=[("xsT", t)])

        do_transposes(0)
        for blk in range(NBLK):
            if blk + 1 < NBLK:
                xr_load(blk + 1)
            xk = [("xsT", t) for t in range(4)]
            for s in range(4):
                n = blk * 4 + s
                if n + 2 < NST:
                    gu_stage_load((n + 2) // 4, (n + 2) % 4, (n + 2) % 3)
                if s == 0:
                    for q in range(4):
                        P.gather(wd[:, q, :, :].rearrange("p f n -> p (f n)"), moe_dn.ap()[:, :], idxdn[:, blk, q:q + 1],
                                 reads=["idxdn"], writes=[("wd", q)], ring="igd", nring=4)
                wb = n % 3
                for fi in range(7):
                    ft = s * 7 + fi
                    pb = (ft % 2) * 2
                    for half in range(2):
                        for kc in range(8):
                            P.op("pe", lambda e, wb=wb, half=half, kc=kc, fi=fi, pb=pb: e.matmul(
                                pp[pb + half][:], wg[wb][:, kc // 4, kc % 4, half * CW + fi * 128:half * CW + (fi + 1) * 128], xsT[:, kc, :],
                                start=(kc == 0), stop=(kc == 7)),
                                reads=xk + [("wg", wb, kc // 4)], writes=[("pp", pb + half)])
                    sgi = ft % 2
                    P.op("act", lambda e, pb=pb, sgi=sgi: e.activation(sg[sgi][:], pp[pb][:], AF.Silu), reads=[("pp", pb)], writes=[("sg", sgi)])
                    P.op("dve", lambda e, pb=pb, sgi=sgi, ft=ft: e.tensor_tensor(act[:, ft, :], sg[sgi][:], pp[pb + 1][:], ALU.mult),
                         reads=[("sg", sgi), ("pp", pb + 1)], writes=[("act", ft)])
            if blk + 1 < NBLK:
                do_transposes(blk + 1)
            actk = [("act", ft) for ft in range(28)]
            for t in range(4):
                yb = ytn % 2
                ytn += 1
                for hh in range(2):
                    for ft in range(28):
                        P.op("pe", lambda e, t=t, hh=hh, ft=ft: e.matmul(py[hh][:], act[:, ft, t * 128:(t + 1) * 128], wd[:, ft // 7, ft % 7, hh * 512:(hh + 1) * 512],
                                                                        start=(ft == 0), stop=(ft == 27)),
                             reads=actk + [("wd", ft // 7)], writes=[("py", hh)])
                    if hh == 0:
                        P.op("act", lambda e, yb=yb: e.activation(yt[yb][:, 0:512], py[0][:], AF.Copy), reads=[("py", 0)], writes=[("yt", yb, 0)])
                    else:
                        P.op("dve", lambda e, yb=yb: e.tensor_copy(yt[yb][:, 512:1024], py[1][:]), reads=[("py", 1)], writes=[("yt", yb, 1)])
                r0 = blk * BLK + t * 128
                P.dma("sp", y_h.ap()[r0:r0 + 128, :], yt[yb][:], reads=[("yt", yb, 0), ("yt", yb, 1)], ring="sty", nring=2)
        P.phase_end()

        P.phase_begin()
        pos_i = P.sb("pos_i", [128, NTT, 2], I32)
        W_all = P.sb("W_all", [128, NTT, 2], F32)
        bcg2 = P.sb("bcg2", [128, D], F32)
        fg = P.sb("fg", [128, D], F32)
        xts = [P.sb("xts%d" % i, [128, D], F32) for i in range(4)]
        y1 = [P.sb("y1_%d" % i, [128, D], F32) for i in range(4)]
        y2 = [P.sb("y2_%d" % i, [128, D], F32) for i in range(4)]
        acc = P.sb("acc", [128, D], F32)
        sq = P.sb("sq", [128, D], BF16)
        ot = [P.sb("ot%d" % i, [128, D], F32) for i in range(4)]
        ssum = P.sb("ssum", [128, 1], F32)
        rstd = P.sb("rstd", [128, 1], F32)
        P.dma("sp", pos_i[:].rearrange("p j k -> p (j k)"), pos_s.ap(), writes=["pos_i"], ring="ld")
        P.dma("sp", W_all[:].rearrange("p j k -> p (j k)"), w_s.ap(), writes=["W_all"], ring="ld")
        P.dma("act", fg[:], final_g.ap().partition_broadcast(128), writes=["fg"], ring="ld")
        sq4 = [sq] + [P.sb("sq%d" % i, [128, D], BF16) for i in range(1, 4)]
        acc2 = [acc] + [P.sb("acc%d" % i, [128, D], F32) for i in range(1, 4)]
        ssum2 = [ssum] + [P.sb("ssum%d" % i, [128, 1], F32) for i in range(1, 4)]
        rstd2 = [rstd] + [P.sb("rstd%d" % i, [128, 1], F32) for i in range(1, 4)]
        bcg2s = [bcg2, P.sb("bcg2B", [128, D], F32)]
        for b in range(NB):
            P.dma("act", bcg2s[b][:], modrows.ap()[b, 1, 5 * D:6 * D].partition_broadcast(128), writes=[("bcg2", b)], ring="ldbc", nring=6)

        def c_front(j):
            b = j // 16
            i2 = j % 4
            ac_ = acc2[i2]
            bcg2 = bcg2s[b]
            P.dma("sp", xts[i2][:], x_s.ap()[j * 128:(j + 1) * 128, :], writes=[("xts", i2)], ring="ldx4", nring=4)
            P.gather(y1[i2][:, :], y_h.ap()[:, :], pos_i[:, j, 0:1], reads=["pos_i"], writes=[("y1", i2)], ring="ig", nring=8)
            P.gather(y2[i2][:, :], y_h.ap()[:, :], pos_i[:, j, 1:2], reads=["pos_i"], writes=[("y2", i2)], ring="ig", nring=8)
            P.op("act", lambda e, i2=i2, j=j, ac_=ac_: e.activation(ac_[:], y1[i2][:], AF.Copy, scale=W_all[:, j, 0:1]), reads=[("y1", i2), "W_all"], writes=[("acc", i2)])
            P.op("dve", lambda e, i2=i2, j=j, ac_=ac_: e.scalar_tensor_tensor(ac_[:], y2[i2][:], W_all[:, j, 1:2], ac_[:], ALU.mult, ALU.add),
                 reads=[("y2", i2), "W_all", ("acc", i2)], writes=[("acc", i2)])
            P.op("dve", lambda e, ac_=ac_, bcg2=bcg2: e.tensor_tensor(ac_[:], ac_[:], bcg2[:], ALU.mult), reads=[("acc", i2), ("bcg2", b)], writes=[("acc", i2)])
            P.op("dve", lambda e, i2=i2, ac_=ac_: e.tensor_tensor(xts[i2][:], xts[i2][:], ac_[:], ALU.add), reads=[("acc", i2), ("xts", i2)], writes=[("xts", i2)])

        def c_back(j):
            i2 = j % 4
            ss_, rs_ = ssum2[i2], rstd2[i2]
            P.op("act", lambda e, i2=i2, ss_=ss_: e.activation(sq4[i2][:], xts[i2][:], AF.Square, accum_out=ss_[:, 0:1]), reads=[("xts", i2)], writes=[("ssum", i2), ("sq", i2)])
            P.op("act", lambda e, ss_=ss_, rs_=rs_: e.activation(rs_[:, 0:1], ss_[:, 0:1], AF.Sqrt, bias=EPS, scale=1.0 / D), reads=[("ssum", i2)], writes=[("rstd", i2)])
            P.op("dve", lambda e, rs_=rs_: e.reciprocal(rs_[:, 0:1], rs_[:, 0:1]), reads=[("rstd", i2)], writes=[("rstd", i2)])
            if upto >= 4:
                P.op("dve", lambda e, i2=i2, rs_=rs_: e.scalar_tensor_tensor(ot[i2][:], xts[i2][:], rs_[:, 0:1], fg[:], ALU.mult, ALU.mult),
                     reads=[("xts", i2), ("rstd", i2), "fg"], writes=[("ot", i2)])
                P.dma("act", out_h.ap()[j * 128:(j + 1) * 128, :], ot[i2][:], reads=[("ot", i2)], ring="sto4", nring=4)
            else:
                P.dma("act", x_s.ap()[j * 128:(j + 1) * 128, :], xts[i2][:], reads=[("xts", i2)], ring="sto4", nring=4)

        c_front(0)
        for j in range(NTT):
            if j + 1 < NTT:
                c_front(j + 1)
            c_back(j)
        P.phase_end()

    if debug:
        P.phase_begin()
        dx = dr("dbg_x", [NB * S, D], F32, kind="ExternalOutput")
        dz = dr("dbg_z", [NB * CT, D], F32, kind="ExternalOutput")
        dm = dr("dbg_mod", [3, 2, 6 * D], F32, kind="ExternalOutput")
        P.dma("sp", dx.ap(), x_s.ap(), ring="st")
        P.dma("sp", dz.ap(), z_s.ap(), ring="st")
        P.dma("sp", dm.ap(), modrows.ap(), ring="st")
        P.phase_end()
    elif upto < 4:
        P.phase_begin()
        P.dma("sp", out_h.ap(), x_s.ap(), ring="st")
        P.phase_end()
    return P.finish()


def _moe_consts():
    mc = np.zeros((128, 632), np.float32)
    p = np.arange(128)
    mc[:, 0:128] = (p[:, None] < p[None, :]).astype(np.float32)
    mc[:, 128:256] = 1.0
    for s_ in range(4):
        for h in range(2):
            mc[:, 256 + s_ * 2 + h] = (p * 4 + s_) * 2 + h
    for q in range(4):
        mc[:, 320 + q] = p * 4 + q
    for blk in range(NBLK):
        mc[:, 348 + blk * 8:348 + (blk + 1) * 8] = BLK * blk
    mc[:, 540:604] = 1024.0
    mc[:, 604:632] = 512.0
    return mc


def _ffn_gu_layout(w):
    w5 = w.reshape(8, 128, 2, 11, 256)
    w5 = w5.transpose(1, 3, 2, 0, 4)
    return np.ascontiguousarray(w5).reshape(128, 11, 2 * 8 * 256)


def _gu_layout(w):
    w6 = w.reshape(NE, 2, 4, 128, 2, 4, 896)
    w6 = w6.transpose(0, 3, 5, 1, 2, 4, 6)
    return np.ascontiguousarray(w6).reshape(NE * 128 * 8, 4 * 2 * 896)


def _dn_layout(w):
    w5 = w.reshape(NE, 4, 7, 128, D)
    w5 = w5.transpose(0, 3, 1, 2, 4)
    return np.ascontiguousarray(w5).reshape(NE * 128 * 4, 7 * D)


def make_in_maps(inputs):
    f = lambda k: np.ascontiguousarray(np.asarray(inputs[k], dtype=np.float32))
    x = f("x"); c = f("c"); ctx = f("ctx"); c_ctx = f("c_ctx")
    atx, atz = _pool_mats()
    conv_w = f("lru_conv_w")[0]; conv_b = f("lru_conv_b")[0]
    b_r = f("lru_b_r")[0]; b_i = f("lru_b_i")[0]; lam = f("lru_lambda")[0]
    pp = np.zeros((1280, 12), np.float32)
    pp[:, 0:4] = conv_w.T
    pp[:, 4] = conv_b
    pp[:, 5:7] = b_r.T
    pp[:, 7:9] = b_i.T
    pp[:, 9:11] = lam.T
    lru_pp = np.ascontiguousarray(pp.reshape(10, 128, 12).transpose(1, 0, 2))
    shared = {
        "ada_w": f("ada_w"), "ada_b": f("ada_b"), "norm1_g": f("norm1_g"), "norm2_g": f("norm2_g"),
        "pool_w": f("pool_w")[0], "pool_scale": f("pool_scale")[0],
        "ffn_w_gu": _ffn_gu_layout(f("ffn_w_gu")[0]), "ffn_w_down": f("ffn_w_down")[0],
        "atx": atx, "atz": atz,
        "lru_w_in": f("lru_w_in")[0], "lru_pp": lru_pp,
        "lru_w_r": f("lru_w_r")[0], "lru_w_i": f("lru_w_i")[0], "lru_w_out": f("lru_w_out")[0],
        "moe_w_router": f("moe_w_router")[0],
        "moe_w_gu": _gu_layout(f("moe_w_gu")[0]), "moe_w_down": _dn_layout(f("moe_w_down")[0]),
        "final_g": f("final_g"), "mconst": _moe_consts(),
    }
    maps = []
    for core in range(8):
        b0 = core * NB
        c3 = np.stack([c[b0], c[b0 + 1], c_ctx], 0)
        cT = np.ascontiguousarray(c3.reshape(3, 8, 128).transpose(2, 1, 0))
        m = dict(shared)
        m["x"] = x[b0:b0 + NB].reshape(NB * S, D)
        m["ctx"] = ctx[b0:b0 + NB].reshape(NB * CT, D)
        m["cT"] = cT
        maps.append(m)
    return maps


_USED = None


def kernel(**inputs):
    nc = build()
    maps = make_in_maps(inputs)
    used = set(nc_input_names(nc))
    maps = [{k: v for k, v in m.items() if k in used} for m in maps]
    res = run_bass_kernel_spmd(nc, maps, core_ids=list(range(8)))
    out = np.stack([r["out"].reshape(NB, S, D) for r in res.results], 0).reshape(16, S, D)
    return out.astype(np.float32)


def nc_input_names(nc):
    return ["x", "ctx", "cT", "ada_w", "ada_b", "norm1_g", "norm2_g", "pool_w", "pool_scale", "ffn_w_gu",
            "ffn_w_down", "atx", "atz", "lru_w_in", "lru_pp", "lru_w_r", "lru_w_i", "lru_w_out",
            "moe_w_router", "moe_w_gu", "moe_w_down", "final_g", "mconst"]
```
